# Optimizing a Trainium2 kernel written in Bass

```python
import math
import jax, jax.numpy as jnp
from jax import lax
import numpy as np


D_MODEL = 2048
BATCH = 4
SEQ = 4096
DEPTH = 4

EXPAND = 2
D_MIX = EXPAND * D_MODEL
N_GROUPS = 4
GROUP = D_MIX // N_GROUPS
RWKV_HEAD = 64
RWKV_HEADS = GROUP // RWKV_HEAD
RWKV_LORA_W = 64
RWKV_LORA_A = 64
RWKV_GN_EPS = 64e-5
FOX_HEAD = 64
FOX_HEADS = GROUP // FOX_HEAD
GDN_HEAD = 128
GDN_HEADS = GROUP // GDN_HEAD
GDN_CONV = 4
GDN_CHUNK = 64
DIFF_HEAD = 64
DIFF_HEADS = GROUP // (2 * DIFF_HEAD)
Q_BLOCK = 128
NORM_EPS = 1e-6

N_RWKV = 3 * GROUP + RWKV_LORA_W + RWKV_LORA_A
N_FOX = 3 * GROUP + FOX_HEADS
N_GDN = 3 * GROUP + 2 * GDN_HEADS
N_DIFF = 3 * GROUP
N_IN = N_RWKV + N_FOX + N_GDN + N_DIFF + D_MIX

kernel_name = 'hybrid_rwkv7_fox_gdn_diff_block'


def rms_norm(x, g, eps=NORM_EPS):
    xf = x.astype(jnp.float32)
    y = xf * lax.rsqrt(jnp.mean(xf * xf, axis=-1, keepdims=True) + eps)
    return (y * g.astype(jnp.float32)).astype(x.dtype)


def l2_normalize(x, eps=1e-6):
    xf = x.astype(jnp.float32)
    return xf * lax.rsqrt(jnp.sum(xf * xf, axis=-1, keepdims=True) + eps)


def block_distance(q0, kv_len):
    return (q0 + jnp.arange(Q_BLOCK))[:, None] - jnp.arange(kv_len)[None, :]


def sweep_query_blocks(block_fn, seq_len):
    outs = [block_fn(i * Q_BLOCK, (i + 1) * Q_BLOCK) for i in range(seq_len // Q_BLOCK)]
    return jnp.concatenate(outs, axis=-2)


def rwkv7_step(state, inp):
    r, w, k, v, a, b = inp
    sa = jnp.einsum('bhij,bhj->bhi', state, a)
    state = state * w[:, :, None, :] + sa[..., None] * b[:, :, None, :] + v[..., None] * k[:, :, None, :]
    return state, jnp.einsum('bhij,bhj->bhi', state, r)


def rwkv7_time_mix(p, mu, w0, w_up, a0, a_up, k_k, k_a, r_k, ln_g, ln_b):
    bsz, seq, _ = p.shape
    f32 = jnp.float32
    prev = jnp.pad(p, ((0, 0), (1, 0), (0, 0)))[:, :-1]
    p = p + mu * (prev - p)
    r, k, v, w_lo, a_lo = jnp.split(p, [GROUP, 2 * GROUP, 3 * GROUP, 3 * GROUP + RWKV_LORA_W], axis=-1)
    w = (w0 + jnp.tanh(w_lo) @ w_up).astype(f32)
    decay = jnp.exp(-jnp.exp(-jax.nn.softplus(-w) - 0.5))
    a = jax.nn.sigmoid((a0 + a_lo @ a_up).astype(f32))
    heads = lambda t: t.astype(f32).reshape(bsz, seq, RWKV_HEADS, RWKV_HEAD)
    kf = k.astype(f32)
    kk = l2_normalize(heads(kf * k_k))
    k_mod = heads(kf * (1.0 + (a - 1.0) * k_a))
    r_h, v_h, a_h, decay_h = heads(r), heads(v), heads(a), heads(decay)
    xs = tuple(jnp.moveaxis(t, 1, 0) for t in (r_h, decay_h, k_mod, v_h, -kk, kk * a_h))
    state0 = jnp.zeros((bsz, RWKV_HEADS, RWKV_HEAD, RWKV_HEAD), f32)
    _, y = lax.scan(rwkv7_step, state0, xs)
    y = jnp.moveaxis(y, 0, 1)
    yc = y - jnp.mean(y, axis=-1, keepdims=True)
    y = yc * lax.rsqrt(jnp.mean(yc * yc, axis=-1, keepdims=True) + RWKV_GN_EPS)
    y = y.reshape(bsz, seq, GROUP) * ln_g + ln_b
    bonus = jnp.sum(r_h * k_mod * r_k, axis=-1, keepdims=True) * v_h
    return (y + bonus.reshape(bsz, seq, GROUP)).astype(p.dtype)


def forgetting_attention(p, f_b, q_g, k_g):
    bsz, seq, _ = p.shape
    f32 = jnp.float32
    q, k, v, f = jnp.split(p, [GROUP, 2 * GROUP, 3 * GROUP], axis=-1)
    shp = (bsz, seq, FOX_HEADS, FOX_HEAD)
    q = rms_norm(q.reshape(shp), q_g).transpose(0, 2, 1, 3)
    k = rms_norm(k.reshape(shp), k_g).transpose(0, 2, 1, 3)
    v = v.reshape(shp).transpose(0, 2, 1, 3)
    log_f = jax.nn.log_sigmoid((f + f_b).astype(f32))
    c = jnp.cumsum(log_f, axis=1).transpose(0, 2, 1)
    scale = FOX_HEAD ** -0.5

    def block(q0, kv_end):
        s = jnp.einsum('bhqd,bhkd->bhqk', q[:, :, q0:q0 + Q_BLOCK], k[:, :, :kv_end],
                       preferred_element_type=f32) * scale
        s = s + c[:, :, q0:q0 + Q_BLOCK, None] - c[:, :, None, :kv_end]
        s = jnp.where(block_distance(q0, kv_end) >= 0, s, -jnp.inf)
        prob = jax.nn.softmax(s, axis=-1)
        return jnp.einsum('bhqk,bhkd->bhqd', prob.astype(v.dtype), v[:, :, :kv_end])

    o = sweep_query_blocks(block, seq)
    return o.transpose(0, 2, 1, 3).reshape(bsz, seq, GROUP)


def causal_depthwise_conv(x, w):
    return lax.conv_general_dilated(x, w.astype(x.dtype)[:, None, :], window_strides=(1,),
                                    padding=[(w.shape[0] - 1, 0)],
                                    dimension_numbers=('NWC', 'WIO', 'NWC'),
                                    feature_group_count=x.shape[-1])


def chunk_gated_delta_rule(q, k, v, g, beta):
    bsz, seq, nh, dk = q.shape
    dv = v.shape[-1]
    n = seq // GDN_CHUNK
    chunks = lambda t: t.reshape(bsz, n, GDN_CHUNK, nh, -1).transpose(0, 3, 1, 2, 4)
    q, k, v = chunks(q), chunks(k), chunks(v)
    g = chunks(g[..., None])[..., 0]
    beta = chunks(beta[..., None])[..., 0]
    gc = jnp.cumsum(g, axis=-1)
    idx = jnp.arange(GDN_CHUNK)
    incl = idx[:, None] >= idx[None, :]
    strict = idx[:, None] > idx[None, :]
    decay = jnp.exp(jnp.where(incl, gc[..., :, None] - gc[..., None, :], -jnp.inf))
    k_beta = k * beta[..., None]
    m = jnp.where(strict, jnp.einsum('bhnid,bhnjd->bhnij', k_beta, k) * decay, 0.0)
    rhs = jnp.concatenate([v * beta[..., None], k_beta * jnp.exp(gc)[..., None]], axis=-1)
    sol = lax.linalg.triangular_solve(jnp.eye(GDN_CHUNK, dtype=m.dtype) + m, rhs,
                                      left_side=True, lower=True)
    u, w = sol[..., :dv], sol[..., dv:]
    a_intra = jnp.einsum('bhnid,bhnjd->bhnij', q, k) * decay

    def step(state, inp):
        q_c, k_c, u_c, w_c, gc_c, a_c = inp
        v_new = u_c - jnp.einsum('bhck,bhkv->bhcv', w_c, state)
        o = (jnp.einsum('bhck,bhkv->bhcv', q_c * jnp.exp(gc_c)[..., None], state)
             + jnp.einsum('bhcj,bhjv->bhcv', a_c, v_new))
        g_last = gc_c[..., -1:]
        state = (state * jnp.exp(g_last)[..., None]
                 + jnp.einsum('bhck,bhcv->bhkv', k_c * jnp.exp(g_last - gc_c)[..., None], v_new))
        return state, o

    xs = tuple(jnp.moveaxis(t, 2, 0) for t in (q, k, u, w, gc, a_intra))
    _, o = lax.scan(step, jnp.zeros((bsz, nh, dk, dv), jnp.float32), xs)
    return o.transpose(1, 0, 3, 2, 4).reshape(bsz, seq, nh, dv)


def gated_deltanet(p, conv_w, a_log, dt_bias, norm_g):
    bsz, seq, _ = p.shape
    f32 = jnp.float32
    qkv, a_logit, b_logit = jnp.split(p, [3 * GROUP, 3 * GROUP + GDN_HEADS], axis=-1)
    qkv = jax.nn.silu(causal_depthwise_conv(qkv, conv_w)).astype(f32)
    q, k, v = (t.reshape(bsz, seq, GDN_HEADS, GDN_HEAD) for t in jnp.split(qkv, 3, axis=-1))
    q = l2_normalize(q) * GDN_HEAD ** -0.5
    k = l2_normalize(k)
    g = -jnp.exp(a_log.astype(f32)) * jax.nn.softplus((a_logit + dt_bias).astype(f32))
    beta = jax.nn.sigmoid(b_logit.astype(f32))
    o = chunk_gated_delta_rule(q, k, v, g, beta)
    return rms_norm(o, norm_g).reshape(bsz, seq, GROUP).astype(p.dtype)


def differential_attention(p, layer, q_g, k_g, lq1, lk1, lq2, lk2, subln_g):
    bsz, seq, _ = p.shape
    f32 = jnp.float32
    q, k, v = jnp.split(p, 3, axis=-1)
    maps = lambda t, g: rms_norm(t.reshape(bsz, seq, DIFF_HEADS, 2, DIFF_HEAD), g).transpose(0, 2, 3, 1, 4)
    q, k = maps(q, q_g), maps(k, k_g)
    v = v.reshape(bsz, seq, DIFF_HEADS, 2 * DIFF_HEAD).transpose(0, 2, 1, 3)
    lam_init = 0.8 - 0.6 * math.exp(-0.3 * layer)
    lam = (jnp.exp(jnp.sum((lq1 * lk1).astype(f32))) - jnp.exp(jnp.sum((lq2 * lk2).astype(f32)))
           + lam_init)
    slopes = jnp.exp2(-8.0 * jnp.arange(1, DIFF_HEADS + 1, dtype=f32) / DIFF_HEADS)
    scale = DIFF_HEAD ** -0.5

    def block(q0, kv_end):
        dist = block_distance(q0, kv_end)
        alibi = -slopes[:, None, None] * dist.astype(f32)
        s = jnp.einsum('bhmqd,bhmkd->bhmqk', q[..., q0:q0 + Q_BLOCK, :], k[..., :kv_end, :],
                       preferred_element_type=f32) * scale + alibi[None, :, None]
        s = jnp.where(dist >= 0, s, -jnp.inf)
        prob = jax.nn.softmax(s, axis=-1)
        weights = prob[:, :, 0] - lam * prob[:, :, 1]
        return jnp.einsum('bhqk,bhkd->bhqd', weights.astype(v.dtype), v[:, :, :kv_end])

    o = sweep_query_blocks(block, seq)
    o = rms_norm(o, subln_g, eps=1e-5) * (1.0 - lam_init)
    return o.transpose(0, 2, 1, 3).reshape(bsz, seq, GROUP)


def setup_inputs(seed: int = 0) -> dict:
    key = jax.random.key(seed)
    ks = iter(jax.random.split(key, 32))
    nrm = lambda shape, s: s * jax.random.normal(next(ks), shape, jnp.float32)
    gain = lambda shape: 1.0 + nrm(shape, 0.02)
    dt = jnp.exp(jax.random.uniform(next(ks), (DEPTH, GDN_HEADS), jnp.float32,
                                    math.log(1e-3), math.log(1e-1)))
    return {
        'x': nrm((BATCH, SEQ, D_MODEL), 1.0),
        'norm_g': gain((DEPTH, D_MODEL)),
        'w_in': nrm((DEPTH, D_MODEL, N_IN), D_MODEL ** -0.5),
        'w_out': nrm((DEPTH, D_MIX, D_MODEL), D_MIX ** -0.5),
        'rwkv_mu': jax.random.uniform(next(ks), (DEPTH, N_RWKV), jnp.float32),
        'rwkv_w0': nrm((DEPTH, GROUP), 1.0),
        'rwkv_w_up': nrm((DEPTH, RWKV_LORA_W, GROUP), 0.1),
        'rwkv_a0': nrm((DEPTH, GROUP), 0.1),
        'rwkv_a_up': nrm((DEPTH, RWKV_LORA_A, GROUP), 0.5 * RWKV_LORA_A ** -0.5),
        'rwkv_k_k': 0.85 + nrm((DEPTH, GROUP), 0.02),
        'rwkv_k_a': gain((DEPTH, GROUP)),
        'rwkv_r_k': nrm((DEPTH, RWKV_HEADS, RWKV_HEAD), 0.1),
        'rwkv_ln_g': gain((DEPTH, GROUP)),
        'rwkv_ln_b': nrm((DEPTH, GROUP), 0.02),
        'fox_q_g': gain((DEPTH, FOX_HEAD)),
        'fox_k_g': gain((DEPTH, FOX_HEAD)),
        'fox_f_b': nrm((DEPTH, FOX_HEADS), 0.1),
        'gdn_conv': nrm((DEPTH, GDN_CONV, 3 * GROUP), GDN_CONV ** -0.5),
        'gdn_a_log': jnp.log(jax.random.uniform(next(ks), (DEPTH, GDN_HEADS), jnp.float32, 1.0, 16.0)),
        'gdn_dt_bias': dt + jnp.log(-jnp.expm1(-dt)),
        'gdn_norm_g': gain((DEPTH, GDN_HEAD)),
        'diff_q_g': gain((DEPTH, DIFF_HEAD)),
        'diff_k_g': gain((DEPTH, DIFF_HEAD)),
        'diff_lq1': nrm((DEPTH, DIFF_HEAD), 0.1),
        'diff_lk1': nrm((DEPTH, DIFF_HEAD), 0.1),
        'diff_lq2': nrm((DEPTH, DIFF_HEAD), 0.1),
        'diff_lk2': nrm((DEPTH, DIFF_HEAD), 0.1),
        'diff_subln_g': gain((DEPTH, 2 * DIFF_HEAD)),
    }


def reference(x, norm_g, w_in, w_out, rwkv_mu, rwkv_w0, rwkv_w_up, rwkv_a0, rwkv_a_up,
              rwkv_k_k, rwkv_k_a, rwkv_r_k, rwkv_ln_g, rwkv_ln_b, fox_q_g, fox_k_g, fox_f_b,
              gdn_conv, gdn_a_log, gdn_dt_bias, gdn_norm_g, diff_q_g, diff_k_g,
              diff_lq1, diff_lk1, diff_lq2, diff_lk2, diff_subln_g):
    bounds = [N_RWKV, N_RWKV + N_FOX, N_RWKV + N_FOX + N_GDN, N_RWKV + N_FOX + N_GDN + N_DIFF]
    for l in range(DEPTH):
        h = rms_norm(x, norm_g[l])
        p = jnp.einsum('bsd,dn->bsn', h, w_in[l])
        p_rwkv, p_fox, p_gdn, p_diff, z = jnp.split(p, bounds, axis=-1)
        y = jnp.concatenate([
            rwkv7_time_mix(p_rwkv, rwkv_mu[l], rwkv_w0[l], rwkv_w_up[l], rwkv_a0[l], rwkv_a_up[l],
                           rwkv_k_k[l], rwkv_k_a[l], rwkv_r_k[l], rwkv_ln_g[l], rwkv_ln_b[l]),
            forgetting_attention(p_fox, fox_f_b[l], fox_q_g[l], fox_k_g[l]),
            gated_deltanet(p_gdn, gdn_conv[l], gdn_a_log[l], gdn_dt_bias[l], gdn_norm_g[l]),
            differential_attention(p_diff, l, diff_q_g[l], diff_k_g[l], diff_lq1[l], diff_lk1[l],
                                   diff_lq2[l], diff_lk2[l], diff_subln_g[l]),
        ], axis=-1)
        x = x + jnp.einsum('bsm,md->bsd', y * jax.nn.silu(z), w_out[l])
    return x
```

```python
import contextlib
import math
import numpy as np
import ml_dtypes
import concourse.bass as bass
import concourse.mybir as mybir
from concourse.bass_utils import run_bass_kernel_spmd

F32 = mybir.dt.float32
BF16 = mybir.dt.bfloat16
ALU = mybir.AluOpType
AF = mybir.ActivationFunctionType
AX = mybir.AxisListType

D_MODEL = 2048
DEPTH = 4
GROUP = 1024
NCH = 2048
EPOCH = 30000
NDMA_SEM = 48
NEG = -30000.0


class Buf:
    __slots__ = ("name", "lw", "rd")

    def __init__(self, name="b"):
        self.name = name
        self.lw = None
        self.rd = []


class Prog:
    ENGS = ("pe", "act", "dve", "pool", "sp")

    def __init__(self, nc):
        self.nc = nc
        self.ops = {e: [] for e in self.ENGS}
        self.ndma = 0
        self.seen = {e: {} for e in self.ENGS}
        self.bar_tiles = None

    def _add(self, eng, fn, reads, writes, dma, extra_waits=()):
        lst = self.ops[eng]
        idx = len(lst)
        waits = []
        seen = self.seen[eng]
        deps = list(extra_waits)
        for b in reads:
            if b.lw is not None:
                deps.append(b.lw)
        for b in writes:
            if b.lw is not None:
                deps.append(b.lw)
            deps.extend(b.rd)
        for d in deps:
            if d[0] == "e":
                _, e2, i2 = d
                if e2 == eng and eng == "pe":
                    continue
                if seen.get(e2, -1) >= i2:
                    continue
                seen[e2] = i2
                self.ops[e2][i2]["signal"] = True
                waits.append(d)
            else:
                did = d[1]
                key = ("d", did % NDMA_SEM)
                if seen.get(key, -1) >= did:
                    continue
                seen[key] = did
                waits.append(d)
        rec = dict(fn=fn, waits=waits, signal=False, dma=None)
        if dma:
            did = self.ndma
            self.ndma += 1
            rec["dma"] = did
            if did >= NDMA_SEM:
                prev = did - NDMA_SEM
                key = ("d", did % NDMA_SEM)
                if seen.get(key, -1) < prev:
                    seen[key] = prev
                    waits.append(("d", prev))
            me = ("d", did)
        else:
            me = ("e", eng, idx)
        lst.append(rec)
        for b in reads:
            if len(b.rd) > 64:
                b.rd = b.rd[-64:]
            b.rd.append(me)
        for b in writes:
            b.lw = me
            b.rd = []
        return me

    def op(self, eng, fn, reads=(), writes=()):
        return self._add(eng, fn, reads, writes, False)

    def dma(self, eng, out, in_, reads=(), writes=(), **kw):
        return self._add(eng, lambda e: e.dma_start(out=out, in_=in_, **kw), reads, writes, True)

    def mm(self, out, lhsT, rhs, start=True, stop=True, reads=(), writes=(), **kw):
        return self.op("pe", lambda e: e.matmul(out, lhsT, rhs, start=start, stop=stop, **kw), reads, writes)

    def barrier(self):
        bt = self.bar_tiles
        hs = []
        hs.append(self.op("pe", lambda e: e.matmul(bt["ps"][0:1, 0:2], bt["c"][0:1, 0:1], bt["c"][0:1, 0:2], start=True, stop=True), reads=[bt["b"]]))
        hs.append(self.op("act", lambda e: e.memzero(bt["a"][0:1, 0:2]), writes=[bt["ba"]]))
        hs.append(self.op("dve", lambda e: e.memset(bt["v"][0:1, 0:2], 0.0), writes=[bt["bv"]]))
        hs.append(self.op("pool", lambda e: e.memset(bt["g"][0:1, 0:2], 0.0), writes=[bt["bg"]]))
        dm = [("d", i) for i in range(max(0, self.ndma - NDMA_SEM), self.ndma)]
        for e in self.ENGS:
            self._add(e, None, (), (), False, extra_waits=[h for h in hs if h[1] != e] + dm)

    def emit(self, final_waits=()):
        nc = self.nc
        if final_waits:
            self._add("sp", None, (), (), False, extra_waits=list(final_waits))
        signo = {}
        nsig = {}
        for e in self.ENGS:
            n = 0
            for i, r in enumerate(self.ops[e]):
                if r["signal"]:
                    signo[(e, i)] = n
                    n += 1
            nsig[e] = n
        self.nsig = nsig
        with contextlib.ExitStack() as st:
            esems = {}
            for e in self.ENGS:
                nep = max(1, (nsig[e] + EPOCH - 1) // EPOCH)
                esems[e] = [st.enter_context(nc.semaphore(f"s_{e}{k}")) for k in range(nep)]
            dsems = [st.enter_context(nc.semaphore(f"s_d{k}")) for k in range(NDMA_SEM)]
            block = st.enter_context(nc.Block())

            def run(e, eng):
                for i, r in enumerate(self.ops[e]):
                    for d in r["waits"]:
                        if d[0] == "e":
                            n = signo[(d[1], d[2])]
                            eng.wait_ge(esems[d[1]][n // EPOCH], (n % EPOCH) + 1)
                        else:
                            did = d[1]
                            eng.wait_ge(dsems[did % NDMA_SEM], 16 * (did // NDMA_SEM + 1))
                    if r["fn"] is None:
                        continue
                    ins = r["fn"](eng)
                    if r["dma"] is not None:
                        ins.then_inc(dsems[r["dma"] % NDMA_SEM], 16)
                    elif r["signal"]:
                        n = signo[(e, i)]
                        ins.then_inc(esems[e][n // EPOCH], 1)

            @block.tensor
            def _(eng):
                run("pe", eng)

            @block.scalar
            def _(eng):
                run("act", eng)

            @block.vector
            def _(eng):
                run("dve", eng)

            @block.gpsimd
            def _(eng):
                run("pool", eng)

            @block.sync
            def _(eng):
                run("sp", eng)
        return {e: len(self.ops[e]) for e in self.ENGS}


_UID = [0]


SB_USE = {"cur": 0, "peak": 0}


def un(name):
    _UID[0] += 1
    return f"{name}_{_UID[0]}"


class Rot:
    def __init__(self, tiles):
        self.t = tiles
        self.b = [Buf() for _ in tiles]
        self.i = 0

    def next(self):
        k = self.i % len(self.t)
        self.i += 1
        return self.t[k], self.b[k]


class Packer:
    def __init__(self):
        self.items = {}
        self.n = 0

    def add(self, name, arr):
        arr = np.asarray(arr, np.float32)
        if arr.ndim == 1:
            arr = arr[:, None]
        assert arr.shape[0] <= 128
        self.items[name] = (self.n, arr)
        self.n += arr.shape[1]

    def layout(self):
        return {k: (off, a.shape[0], a.shape[1]) for k, (off, a) in self.items.items()}

    def build(self):
        out = np.zeros((128, self.n), np.float32)
        for k, (off, a) in self.items.items():
            out[: a.shape[0], off:off + a.shape[1]] = a
        return out


def col_groups():
    g = []
    for i in range(13):
        g.append(("rwkv", i))
    for i in range(4):
        g.append(("qk", ("fq", i)))
    for i in range(4):
        g.append(("qk", ("fk", i)))
    for i in range(4):
        g.append(("v", ("fv", i)))
    g.append(("misc", ("f", 8)))
    for i in range(12):
        g.append(("raw", i))
    g.append(("misc", ("a", 4)))
    g.append(("misc", ("b", 4)))
    for i in range(4):
        g.append(("qk", ("dq", i)))
    for i in range(4):
        g.append(("qk", ("dk", i)))
    for i in range(4):
        g.append(("v", ("dv", i)))
    for i in range(16):
        g.append(("z", i))
    return g


def core_columns(hh):
    G = GROUP
    h5 = hh * 512
    n_rwkv = 3 * G + 128
    n_fox = 3 * G + 16
    n_gdn = 3 * G + 16
    o_f = n_rwkv
    o_g = o_f + n_fox
    o_d = o_g + n_gdn
    o_z = o_d + 3 * G
    cols = []
    r = np.concatenate([np.arange(h5, h5 + 512), G + np.arange(h5, h5 + 512), 2 * G + np.arange(h5, h5 + 512),
                        3 * G + np.arange(128)])
    for i in range(13):
        cols.append(r[i * 128:(i + 1) * 128])
    for part in range(3):
        c = o_f + part * G + np.arange(h5, h5 + 512)
        for i in range(4):
            cols.append(c[i * 128:(i + 1) * 128])
    cols.append(o_f + 3 * G + np.arange(hh * 8, hh * 8 + 8))
    c = np.concatenate([o_g + part * G + np.arange(h5, h5 + 512) for part in range(3)])
    for i in range(12):
        cols.append(c[i * 128:(i + 1) * 128])
    cols.append(o_g + 3 * G + np.arange(hh * 4, hh * 4 + 4))
    cols.append(o_g + 3 * G + 8 + np.arange(hh * 4, hh * 4 + 4))
    for part in range(3):
        c = o_d + part * G + np.arange(h5, h5 + 512)
        for i in range(4):
            cols.append(c[i * 128:(i + 1) * 128])
    zc = np.concatenate([o_z + m * G + np.arange(h5, h5 + 512) for m in range(4)])
    for i in range(16):
        cols.append(zc[i * 128:(i + 1) * 128])
    return cols


def my_channels(hh):
    return np.concatenate([m * GROUP + np.arange(hh * 512, hh * 512 + 512) for m in range(4)])


def pack_params(inp, l, hh, S):
    pk = Packer()
    f = lambda k: np.asarray(inp[k][l], np.float32)
    pk.add("norm_g", f("norm_g").reshape(16, 128).T)
    h5 = slice(hh * 512, hh * 512 + 512)
    mu = f("rwkv_mu")
    mu_my = np.concatenate([mu[0:1024][h5], mu[1024:2048][h5], mu[2048:3072][h5], mu[3072:3200]])
    pk.add("mu", mu_my.reshape(13, 128).T)
    perhead = lambda v: v[h5].reshape(8, 64).T
    pk.add("w0", perhead(f("rwkv_w0")))
    pk.add("a0", perhead(f("rwkv_a0")))
    pk.add("k_k", perhead(f("rwkv_k_k")))
    pk.add("k_a", perhead(f("rwkv_k_a")))
    pk.add("r_k", perhead(f("rwkv_r_k").reshape(-1)))
    pk.add("ln_g", perhead(f("rwkv_ln_g")))
    pk.add("ln_b", perhead(f("rwkv_ln_b")))
    pk.add("w_up", f("rwkv_w_up")[:, h5])
    pk.add("a_up", f("rwkv_a_up")[:, h5])
    sc = 64 ** -0.5
    pk.add("fq_g", np.tile(f("fox_q_g"), 2))
    pk.add("fk_g", np.tile(f("fox_k_g"), 2))
    pk.add("fq_row", f("fox_q_g")[None, :])
    pk.add("fk_row", f("fox_k_g")[None, :])
    pk.add("f_b", f("fox_f_b")[hh * 8: hh * 8 + 8])
    pk.add("dq_g", np.tile(f("diff_q_g"), 2))
    pk.add("dk_g", np.tile(f("diff_k_g"), 2))
    pk.add("dq_row", f("diff_q_g")[None, :])
    pk.add("dk_row", f("diff_k_g")[None, :])
    pk.add("dl", np.stack([f("diff_lq1"), f("diff_lk1"), f("diff_lq2"), f("diff_lk2")], 1))
    pk.add("subln", f("diff_subln_g"))
    lam_init = 0.8 - 0.6 * math.exp(-0.3 * l)
    pk.add("laminit", np.full((128, 1), lam_init))
    pk.add("oneml", np.full((128, 1), 1.0 - lam_init))
    cw = f("gdn_conv")
    cw_my = np.concatenate([cw[:, part * 1024:(part + 1) * 1024][:, h5] for part in range(3)], 1)
    pk.add("conv", cw_my.reshape(4, 12, 128).transpose(2, 1, 0).reshape(128, 48))
    pk.add("a_log", f("gdn_a_log")[hh * 4: hh * 4 + 4])
    pk.add("dt_b", f("gdn_dt_bias")[hh * 4: hh * 4 + 4])
    pk.add("gnorm", f("gdn_norm_g"))
    pk.add("ident", np.eye(128))
    pk.add("ones", np.ones((128, 128)))
    bo = np.zeros((128, 128))
    bo[:64, :64] = 1
    bo[64:, 64:] = 1
    pk.add("blockones", bo)
    kk, qq = np.meshgrid(np.arange(128), np.arange(128), indexing="ij")
    pk.add("cmask", np.where(kk > qq, NEG, 0.0))
    ii, jj = np.meshgrid(np.arange(64), np.arange(64), indexing="ij")
    pk.add("mSU", (ii < jj).astype(np.float32))
    pk.add("mSL", (jj < ii).astype(np.float32))
    pk.add("mIU", (ii <= jj).astype(np.float32))
    pk.add("nMUI", np.where(ii <= jj, 0.0, NEG))
    pk.add("nMUS", np.where(ii < jj, 0.0, NEG))
    pk.add("nMLS", np.where(jj < ii, 0.0, NEG))
    E1 = np.zeros((4, 8)); E1[:, 0:4] = np.eye(4)
    E2 = np.zeros((4, 8)); E2[:, 4:8] = -np.eye(4)
    pk.add("E1", E1)
    pk.add("E2", E2)
    pk.add("c1", np.array([0, 0, 0, 0, 1, 1, 1, 1.0]))
    pk.add("c2", np.array([1, 1, 1, 1, 0, 0, 0, 0.0]))
    hm = np.zeros((8, 4))
    for h_ in range(4):
        hm[h_, h_] = 1
        hm[4 + h_, h_] = 1
    pk.add("hmask", hm)
    sel = np.zeros((4, 4 * 128))
    for h_ in range(4):
        sel[h_, h_ * 128:(h_ + 1) * 128] = 1
    pk.add("sel", sel)
    rm = np.ones((128, 64), np.float32)
    rm[:, 0] = 0.0
    pk.add("rmask", rm)
    slopes = 2.0 ** (-8.0 * np.arange(1, 9) / 8)
    sl = slopes[hh * 4: hh * 4 + 4]
    t = np.arange(S, dtype=np.float64)
    pk.add("alibi_tok", (t.reshape(S // 128, 128).T[:, :, None] * sl[None, None, :]).reshape(128, -1))
    return pk


class Ctx:
    pass


def build_program(S, lidx, has_prev, front, layout, debug_mixers=("fox", "diff", "rwkv", "gdn")):
    nc = bass.Bass("TRN2", target_bir_lowering=False)
    C = Ctx()
    C.nc, C.S, C.lidx = nc, S, lidx
    P = Prog(nc)
    C.P = P
    NG = len(col_groups())
    C.NG = NG
    d_in = lambda name, shape, dt: nc.dram_tensor(name, list(shape), dt, kind="ExternalInput").ap()
    d_out = lambda name, shape, dt: nc.dram_tensor(name, list(shape), dt, kind="ExternalOutput").ap()
    d_tmp = lambda name, shape, dt: nc.dram_tensor(name, list(shape), dt).ap()
    C.xT_in = d_in("xT", [D_MODEL, S], F32)
    finals = []
    with contextlib.ExitStack() as st:
        C.st = st
        bt = dict(
            ps=st.enter_context(nc.psum_tensor("bar_ps", [128, 512], F32)),
            c=st.enter_context(nc.sbuf_tensor("bar_c", [128, 16], BF16)),
            a=st.enter_context(nc.sbuf_tensor("bar_a", [128, 16], F32)),
            v=st.enter_context(nc.sbuf_tensor("bar_v", [128, 16], F32)),
            g=st.enter_context(nc.sbuf_tensor("bar_g", [128, 16], F32)),
        )
        bt["b"] = Buf("bar")
        bt["ba"], bt["bv"], bt["bg"] = Buf(), Buf(), Buf()
        P.bar_tiles = bt
        P.op("dve", lambda e: e.memset(bt["c"][:], 0.0), writes=[bt["b"]])
        P.barrier()
        xcur = C.xT_in
        if has_prev:
            C.yzp = d_in("yzp", [2 * NCH, S], BF16)
            C.wout = d_in("wout", [16, 128, 32, 128], F32)
            C.xT_out = d_out("xTo", [D_MODEL, S], F32)
            finals += phase_outproj(C)
            P.barrier()
            xcur = C.xT_out
        if front:
            C.xcur = xcur
            C.win = d_in("win", [NG, 128, 16, 128], F32)
            ncol = max(o + c for (o, r, c) in layout.values())
            C.prm_d = d_in("prm", [128, ncol], F32)
            C.layout = layout
            C.yz = d_out("yz", [NCH, S], BF16)
            C.rwkvT = d_tmp("rwkvT", [1664, S], F32)
            C.gdnT = d_tmp("gdnT", [1536, S], F32)
            C.zT = d_tmp("zT", [NCH, S], F32)
            C.fq = d_tmp("fq", [8, 67, S], BF16)
            C.fk = d_tmp("fk", [8, 67, S], BF16)
            C.fv = d_tmp("fv", [S, 8, 65], BF16)
            C.dq = d_tmp("dq", [8, 67, S], BF16)
            C.dk = d_tmp("dk", [8, 67, S], BF16)
            C.dv = d_tmp("dv", [S, 4, 128], BF16)
            sb = lambda name, shape, dt: st.enter_context(nc.sbuf_tensor(un(name), list(shape), dt))
            C.sb = sb
            C.prm = sb("prm_sb", [128, ncol], F32)
            C.b_prm = Buf("prm")
            P.dma("sp", C.prm[:], C.prm_d, writes=[C.b_prm])
            C.identb = sb("identb", [128, 128], BF16)
            C.onesb = sb("onesb", [128, 128], BF16)
            C.bonesb = sb("bonesb", [128, 128], BF16)
            C.cmaskb = sb("cmaskb", [128, 128], BF16)
            C.b_const = Buf("const")
            for nm, tl in (("ident", C.identb), ("ones", C.onesb), ("blockones", C.bonesb), ("cmask", C.cmaskb)):
                P.op("dve", lambda e, nm=nm, tl=tl: e.tensor_copy(tl[:], prm_ap(C, nm)), reads=[C.b_prm], writes=[C.b_const])
            C.epsc = sb("epsc", [128, 4], F32)
            C.epsi = {1e-6: 0, 1e-5: 1, 64e-5: 2, 1.0: 3}
            for ev, ei in C.epsi.items():
                P.op("pool", lambda e, ev=ev, ei=ei: e.memset(C.epsc[:, ei:ei + 1], ev), writes=[C.b_const])
            C.miscT = d_tmp("miscT", [3, 8, S], F32)
            C.alibi_d = d_in("alibi", [4, S], F32)
            C.yz_w = []
            phase_inproj(C)
            P.barrier()
            if "rwkv" in debug_mixers or "gdn" in debug_mixers:
                setup_chunk_consts(C)
            if "rwkv" in debug_mixers:
                phase_rwkv(C)
                P.barrier()
            if "gdn" in debug_mixers:
                phase_gdn(C)
                P.barrier()
            if "fox" in debug_mixers:
                phase_attn(C, "fox")
                P.barrier()
            if "diff" in debug_mixers:
                phase_attn(C, "diff")
                P.barrier()
            finals += C.yz_w
        counts = P.emit(final_waits=finals[-NDMA_SEM:])
    C.counts = counts
    return nc, C


def prm_ap(C, name, rows=None, c0=0, c1=None):
    off, r, c = C.layout[name]
    if c1 is None:
        c1 = c
    if rows is None:
        rows = (0, r)
    return C.prm[rows[0]:rows[1], off + c0: off + c1]


def rsqrt_from(C, out, in_, scale, eps, b_in, b_out):
    P = C.P
    P.op("act", lambda e: e.activation(out, in_, AF.Sqrt, bias=C.epsc[0:out.shape[0], C.epsi[eps]:C.epsi[eps] + 1], scale=scale), reads=[b_in, C.b_const], writes=[b_out])
    P.op("dve", lambda e: e.reciprocal(out, out), reads=[b_out], writes=[b_out])


def phase_inproj(C):
    nc, P, S = C.nc, C.P, C.S
    HS = S // 2
    NT = HS // 512
    groups = col_groups()
    with contextlib.ExitStack() as st:
        sb = lambda name, shape, dt: st.enter_context(nc.sbuf_tensor(un(name), list(shape), dt))
        ps = lambda name: st.enter_context(nc.psum_tensor(un(name), [128, 512], F32))
        hT = sb("hT", [128, 16, HS], BF16)
        b_hT = Buf("hT")
        XW = 128
        xts = Rot([sb(f"xt{i}", [128, 16, XW], F32) for i in range(2)])
        sqs = Rot([sb(f"sq{i}", [128, 16, XW], BF16) for i in range(1)])
        rstds = Rot([sb(f"rstd{i}", [128, XW], F32) for i in range(2)])
        stg = Rot([sb(f"wst{i}", [128, 16, 128], F32) for i in range(3)])
        wbs = Rot([sb(f"wb{i}", [128, 16, 128], BF16) for i in range(2)])
        pacc = Rot([ps(f"pacc{i}") for i in range(4)])
        pstat = Rot([ps(f"pstat{i}") for i in range(2)])
        pf = Rot([sb(f"pf{i}", [128, 516], F32) for i in range(2)])
        tmp = Rot([sb(f"tmp{i}", [128, 512], F32) for i in range(2)])
        of32 = Rot([sb(f"of{i}", [128, 512], F32) for i in range(3)])
        obf = Rot([sb(f"ob{i}", [128, 512], BF16) for i in range(3)])
        sqb = Rot([sb(f"sqb{i}", [128, 512], BF16) for i in range(2)])
        rs2 = Rot([sb(f"rs2{i}", [128, 512], F32) for i in range(2)])
        omu = sb("omu", [128, 13], F32)
        b_omu = Buf()
        last = sb("lastcol", [128, 13], F32)
        b_last = Buf()
        vfox = Rot([sb(f"vfox{i}", [128, 4, 2, 65], BF16) for i in range(2)])
        vdif = Rot([sb(f"vdif{i}", [128, 4, 128], BF16) for i in range(2)])
        gq = sb("gqs", [128, 4], F32)
        b_gq = Buf()
        sc = 64 ** -0.5
        for j, (nm, s_) in enumerate((("fq_g", sc), ("fk_g", 1.0), ("dq_g", sc), ("dk_g", 1.0))):
            P.op("dve", lambda e, j=j, nm=nm, s_=s_: e.tensor_scalar_mul(gq[:, j:j + 1], prm_ap(C, nm), s_),
                 reads=[C.b_prm], writes=[b_gq])
        P.op("dve", lambda e: e.tensor_scalar(omu[:], prm_ap(C, "mu"), -1.0, 1.0, ALU.mult, ALU.add),
             reads=[C.b_prm], writes=[b_omu])
        P.op("dve", lambda e: e.memset(last[:], 0.0), writes=[b_last])
        for t_, b_ in zip(vfox.t, vfox.b):
            P.op("pool", lambda e, t_=t_: e.memset(t_[:], 1.0), writes=[b_])
        wq = {}

        def load_w(gi):
            t, b = stg.next()
            P.dma("sp", t[:], C.win[gi], writes=[b])
            wq[gi] = (t, b)

        wc = {}

        def cast_w(gi):
            t, b = wq.pop(gi)
            tb, bb = wbs.next()
            P.op("pool", lambda e: e.tensor_copy(tb[:], t[:]), reads=[b], writes=[bb])
            wc[gi] = (tb, bb)

        for half in range(2):
            h0 = half * HS
            for tt in range(HS // XW):
                t0 = h0 + tt * XW
                xt, bx = xts.next()
                P.dma("sp", xt[:], C.xcur[:, t0:t0 + XW].rearrange("(c p) t -> p c t", p=128), writes=[bx])
                sq, bsq = sqs.next()
                P.op("act", lambda e, sq=sq, xt=xt: e.activation(sq[:], xt[:], AF.Square), reads=[bx], writes=[bsq])
                pst, bps = pstat.next()
                for c in range(16):
                    P.mm(pst[:, 0:XW], C.onesb[:], sq[:, c, :], start=(c == 0), stop=(c == 15),
                         reads=[bsq, C.b_const], writes=[bps])
                rs, brs = rstds.next()
                rsqrt_from(C, rs[:], pst[:, 0:XW], 1.0 / D_MODEL, 1e-6, bps, brs)
                for c in range(16):
                    P.op("dve", lambda e, c=c, xt=xt, rs=rs, tt=tt: e.scalar_tensor_tensor(
                        hT[:, c, tt * XW:(tt + 1) * XW], xt[:, c, :], prm_ap(C, "norm_g", c0=c, c1=c + 1), rs[:],
                        ALU.mult, ALU.mult), reads=[bx, brs, C.b_prm], writes=[b_hT])
            load_w(0)
            load_w(1)
            cast_w(0)
            for gi, (kind, meta) in enumerate(groups):
                if gi + 2 < len(groups):
                    load_w(gi + 2)
                if gi + 1 < len(groups):
                    cast_w(gi + 1)
                wb, bwb = wc.pop(gi)
                if kind == "v":
                    which, i = meta
                    for t4 in range(HS // 512):
                        pa, bpa = pacc.next()
                        for tb in range(4):
                            tk = t4 * 512 + tb * 128
                            for c in range(16):
                                P.mm(pa[:, tb * 128:(tb + 1) * 128], hT[:, c, tk:tk + 128], wb[:, c, :],
                                     start=(c == 0), stop=(c == 15), reads=[b_hT, bwb], writes=[bpa])
                        tg = h0 + t4 * 512
                        if which == "fv":
                            vt, bv = vfox.next()
                            P.op("act", lambda e, vt=vt, pa=pa: e.activation(
                                vt[:, :, :, 0:64], pa[:].rearrange("p (a h d) -> p a h d", a=4, h=2), AF.Copy),
                                reads=[bpa], writes=[bv])
                            P.dma("act", C.fv[tg:tg + 512, 2 * i:2 * i + 2, :].rearrange("(a p) h d -> p a h d", p=128),
                                  vt[:], reads=[bv])
                        else:
                            vt, bv = vdif.next()
                            P.op("act", lambda e, vt=vt, pa=pa: e.activation(
                                vt[:], pa[:].rearrange("p (a d) -> p a d", a=4), AF.Copy), reads=[bpa], writes=[bv])
                            P.dma("act", C.dv[tg:tg + 512, i, :].rearrange("(a p) d -> p a d", p=128), vt[:], reads=[bv])
                    continue
                for tt in range(NT):
                    tl = tt * 512
                    tg = h0 + tl
                    pa, bpa = pacc.next()
                    for c in range(16):
                        P.mm(pa[:], wb[:, c, :], hT[:, c, tl:tl + 512], start=(c == 0), stop=(c == 15),
                             reads=[b_hT, bwb], writes=[bpa])
                    if kind == "rwkv":
                        i = meta
                        pft, bpf = pf.next()
                        first = (half == 0 and tt == 0)
                        if tt == 0:
                            P.op("act", lambda e, pft=pft, i=i: e.copy(pft[:, 0:1], last[:, i:i + 1]), reads=[b_last], writes=[bpf])
                        else:
                            pprev, bprev = pf.t[(pf.i - 2) % 2], pf.b[(pf.i - 2) % 2]
                            P.op("act", lambda e, pft=pft, pprev=pprev: e.copy(pft[:, 0:1], pprev[:, 512:513]), reads=[bprev], writes=[bpf])
                        P.op("act", lambda e, pft=pft, pa=pa: e.copy(pft[:, 1:513], pa[:]), reads=[bpa], writes=[bpf])
                        if tt == NT - 1:
                            P.op("act", lambda e, pft=pft, i=i: e.copy(last[:, i:i + 1], pft[:, 512:513]), reads=[bpf], writes=[b_last])
                        tm, btm = tmp.next()
                        P.op("dve", lambda e, tm=tm, pft=pft, i=i: e.tensor_scalar_mul(tm[:], pft[:, 0:512], prm_ap(C, "mu", c0=i, c1=i + 1)),
                             reads=[bpf, C.b_prm], writes=[btm])
                        o, bo = of32.next()
                        P.op("dve", lambda e, o=o, pft=pft, tm=tm, i=i: e.scalar_tensor_tensor(
                            o[:], pft[:, 1:513], omu[:, i:i + 1], tm[:], ALU.mult, ALU.add), reads=[bpf, btm, b_omu], writes=[bo])
                        P.dma("sp", C.rwkvT[i * 128:(i + 1) * 128, tg:tg + 512], o[:], reads=[bo])
                    elif kind == "qk":
                        which, i = meta
                        gcol = {"fq": 0, "fk": 1, "dq": 2, "dk": 3}[which]
                        dst = {"fq": C.fq, "fk": C.fk, "dq": C.dq, "dk": C.dk}[which]
                        sq2, bsq2 = sqb.next()
                        P.op("act", lambda e, sq2=sq2, pa=pa: e.activation(sq2[:], pa[:], AF.Square), reads=[bpa], writes=[bsq2])
                        pst, bps = pstat.next()
                        P.mm(pst[:], C.bonesb[:], sq2[:], reads=[bsq2, C.b_const], writes=[bps])
                        r2, br2 = rs2.next()
                        rsqrt_from(C, r2[:], pst[:], 1.0 / 64, 1e-6, bps, br2)
                        o, bo = obf.next()
                        P.op("dve", lambda e, o=o, pa=pa, r2=r2, gcol=gcol: e.scalar_tensor_tensor(
                            o[:], pa[:], gq[:, gcol:gcol + 1], r2[:], ALU.mult, ALU.mult), reads=[bpa, br2, b_gq], writes=[bo])
                        P.dma("sp", dst[2 * i, 0:64, tg:tg + 512], o[0:64, :], reads=[bo])
                        P.dma("sp", dst[2 * i + 1, 0:64, tg:tg + 512], o[64:128, :], reads=[bo])
                    elif kind == "raw":
                        i = meta
                        o, bo = of32.next()
                        P.op("act", lambda e, o=o, pa=pa: e.copy(o[:], pa[:]), reads=[bpa], writes=[bo])
                        P.dma("act", C.gdnT[i * 128:(i + 1) * 128, tg:tg + 512], o[:], reads=[bo])
                    elif kind == "z":
                        i = meta
                        o, bo = of32.next()
                        P.op("act", lambda e, o=o, pa=pa: e.activation(o[:], pa[:], AF.Silu), reads=[bpa], writes=[bo])
                        P.dma("act", C.zT[i * 128:(i + 1) * 128, tg:tg + 512], o[:], reads=[bo])
                    elif kind == "misc":
                        which, n = meta
                        mi = {"f": 0, "a": 1, "b": 2}[which]
                        o, bo = of32.next()
                        P.op("act", lambda e, o=o, pa=pa, n=n: e.copy(o[0:n, :], pa[0:n, :]), reads=[bpa], writes=[bo])
                        P.dma("act", C.miscT[mi, 0:n, tg:tg + 512], o[0:n, :], reads=[bo])


def phase_attn(C, which):
    nc, P, S = C.nc, C.P, C.S
    NB = S // 128
    NQ = S // 512
    fox = which == "fox"
    nu = 8 if fox else 4
    dvv = 65 if fox else 128
    qd, kd = (C.fq, C.fk) if fox else (C.dq, C.dk)
    with contextlib.ExitStack() as st:
        sb = lambda name, shape, dt: st.enter_context(nc.sbuf_tensor(un(name), list(shape), dt))
        ps = lambda name: st.enter_context(nc.psum_tensor(un(name), [128, 512], F32))
        nh = 8 if fox else 4
        negB = sb("negB", [128, 1], F32)
        bias = sb("bias", [128, NB * nh], F32)
        lam = sb("lam", [128, 4], F32)
        gsub = sb("gsub", [128, 1], F32)
        pmisc = ps("pmisc")
        st2 = contextlib.ExitStack()
        sbp = lambda name, shape, dt: st2.enter_context(nc.sbuf_tensor(un(name), list(shape), dt))
        cneg = sbp("cneg", [nh, S], F32)
        b_c = Buf()
        b_pm = Buf()
        if fox:
            e1 = sbp("e1", [nh, S], F32)
            nfb = sbp("nfb", [nh, 1], F32)
            onesr = sbp("onesr", [nh, 512], F32)
            b_e1, b_nfb, b_on = Buf(), Buf(), Buf()
            P.dma("sp", e1[:], C.miscT[0, 0:8, :], writes=[b_e1])
            P.op("dve", lambda e: e.tensor_scalar_mul(nfb[:], prm_ap(C, "f_b"), -1.0), reads=[C.b_prm], writes=[b_nfb])
            P.op("pool", lambda e: e.memset(onesr[:], 1.0), writes=[b_on])
            P.op("act", lambda e: e.activation(e1[:], e1[:], AF.Exp, bias=nfb[:], scale=-1.0), reads=[b_e1, b_nfb], writes=[b_e1])
            P.op("act", lambda e: e.activation(e1[:], e1[:], AF.Ln, bias=C.epsc[0:nh, 3:4], scale=1.0), reads=[b_e1, C.b_const], writes=[b_e1])
            for qi in range(S // 512):
                ini = 0.0 if qi == 0 else cneg[:, qi * 512 - 1: qi * 512]
                P.op("dve", lambda e, qi=qi, ini=ini: e.tensor_tensor_scan(cneg[:, qi * 512:(qi + 1) * 512], onesr[:], e1[:, qi * 512:(qi + 1) * 512], ini, ALU.mult, ALU.add),
                     reads=[b_e1, b_on, b_c], writes=[b_c])
        else:
            P.dma("sp", cneg[:], C.alibi_d, writes=[b_c])
        pc = [sbp(f"pc{i}", [nh, S], BF16) for i in range(3)]
        rr = sbp("rres", [nh, S], F32)
        r2 = sbp("rres2", [nh, S], F32)
        b_pc, b_rr, b_r2 = Buf(), Buf(), Buf()
        P.op("dve", lambda e: e.tensor_scalar_mul(rr[:], cneg[:], -1.0), reads=[b_c], writes=[b_rr])
        for i in range(3):
            P.op("dve", lambda e, i=i: e.tensor_copy(pc[i][:], rr[:]), reads=[b_rr], writes=[b_pc])
            if i < 2:
                P.op("dve", lambda e, i=i: e.tensor_copy(r2[:], pc[i][:]), reads=[b_pc], writes=[b_r2])
                P.op("dve", lambda e: e.tensor_sub(rr[:], rr[:], r2[:]), reads=[b_rr, b_r2], writes=[b_rr])
        onesk = sbp("onesk", [nh, S], BF16)
        b_ok = Buf()
        P.op("pool", lambda e: e.memset(onesk[:], 1.0), writes=[b_ok])
        b_qd, b_kd = Buf("qd"), Buf("kd")
        for i in range(3):
            if fox:
                P.dma("sp", qd[:, 64 + i, :], pc[i][:], reads=[b_pc], writes=[b_qd])
                P.dma("sp", kd[:, 64 + i, :], onesk[:], reads=[b_ok], writes=[b_kd])
            else:
                for m in range(2):
                    P.dma("sp", qd.rearrange("(h m) r s -> h m r s", m=2)[:, m, 64 + i, :], pc[i][:], reads=[b_pc], writes=[b_qd])
                    P.dma("sp", kd.rearrange("(h m) r s -> h m r s", m=2)[:, m, 64 + i, :], onesk[:], reads=[b_ok], writes=[b_kd])
        mx = sbp("mx", [1, 4], F32)
        b_mx = Buf()
        qn, kn = ("fq_row", "fk_row") if fox else ("dq_row", "dk_row")
        P.op("dve", lambda e: e.tensor_reduce(mx[:, 0:1], prm_ap(C, qn), AX.X, ALU.max, apply_absolute_value=True), reads=[C.b_prm], writes=[b_mx])
        P.op("dve", lambda e: e.tensor_reduce(mx[:, 1:2], prm_ap(C, kn), AX.X, ALU.max, apply_absolute_value=True), reads=[C.b_prm], writes=[b_mx])
        P.op("dve", lambda e: e.scalar_tensor_tensor(mx[:, 2:3], mx[:, 0:1], -8.0, mx[:, 1:2], ALU.mult, ALU.mult), reads=[b_mx], writes=[b_mx])
        b_nB = Buf()
        P.mm(pmisc[:, 0:1], prm_ap(C, "ones", rows=(0, 1)), mx[:, 2:3], reads=[b_mx, C.b_prm], writes=[b_pm])
        P.op("dve", lambda e: e.tensor_copy(negB[:], pmisc[:, 0:1]), reads=[b_pm], writes=[b_nB])
        b_bias = Buf()
        if fox:
            for blk in range(NB):
                P.op("pe", lambda e, blk=blk: e.transpose(pmisc[:, 8 + blk * nh: 8 + (blk + 1) * nh], cneg[:, blk * 128:(blk + 1) * 128],
                                                          prm_ap(C, "ident", rows=(0, nh), c1=nh)), reads=[b_c, C.b_prm], writes=[b_pm])
            P.op("dve", lambda e: e.tensor_scalar(bias[:], pmisc[:, 8:8 + NB * nh], negB[:], None, ALU.add), reads=[b_pm, b_nB], writes=[b_bias])
        else:
            P.op("dve", lambda e: e.tensor_scalar(bias[:], prm_ap(C, "alibi_tok"), negB[:], None, ALU.add), reads=[C.b_prm, b_nB], writes=[b_bias])
        biasv = bias[:].rearrange("p (b h) -> p b h", h=nh)
        if not fox:
            pr = sbp("lprod", [64, 2], F32)
            b_pr = Buf()
            P.op("dve", lambda e: e.tensor_mul(pr[:, 0:1], prm_ap(C, "dl", c0=0, c1=1), prm_ap(C, "dl", c0=1, c1=2)), reads=[C.b_prm], writes=[b_pr])
            P.op("dve", lambda e: e.tensor_mul(pr[:, 1:2], prm_ap(C, "dl", c0=2, c1=3), prm_ap(C, "dl", c0=3, c1=4)), reads=[C.b_prm], writes=[b_pr])
            P.mm(pmisc[:, 2:4], prm_ap(C, "ones", rows=(0, 64)), pr[:], reads=[b_pr, C.b_prm, b_nB], writes=[b_pm])
            b_lam = Buf()
            P.op("act", lambda e: e.activation(lam[:, 0:2], pmisc[:, 2:4], AF.Exp), reads=[b_pm], writes=[b_lam])
            P.op("dve", lambda e: e.tensor_sub(lam[:, 2:3], lam[:, 1:2], lam[:, 0:1]), reads=[b_lam], writes=[b_lam])
            P.op("dve", lambda e: e.tensor_scalar(lam[:, 3:4], lam[:, 2:3], prm_ap(C, "laminit"), None, ALU.subtract), reads=[b_lam, C.b_prm], writes=[b_lam])
            P.op("dve", lambda e: e.tensor_scalar(gsub[:], prm_ap(C, "subln"), prm_ap(C, "oneml"), None, ALU.mult), reads=[C.b_prm], writes=[b_lam])
        P.barrier()
        st2.close()
        kts = Rot([sb(f"kt{i}", [67, S], BF16) for i in range(2)])
        qts = Rot([sb(f"qt{i}", [67, 512], BF16) for i in range(3)])
        vts = Rot([sb(f"vt{i}", [128, NB, dvv], BF16) for i in range(2)])
        pts = Rot([sb(f"pT{i}", [128, 512], BF16) for i in range(4)])
        pS = Rot([ps(f"pS{i}") for i in range(3 if fox else 2)])
        if fox:
            pO = Rot([ps(f"pO{i}") for i in range(2)])
        else:
            pO = Rot([ps(f"pO{i}") for i in range(4)])
        zts = Rot([sb(f"zt{i}", [128, 512], F32) for i in range(2)])
        osb = Rot([sb(f"osb{i}", [128, 512], F32) for i in range(4)])
        rsb = Rot([sb(f"rsb{i}", [128, 512], F32) for i in range(3)])
        ybf = Rot([sb(f"ybf{i}", [128, 512], BF16) for i in range(2)])
        sq3 = Rot([sb(f"sq3{i}", [128, 512], BF16) for i in range(2)])
        ch0 = 512 if fox else 1536

        def scores_and_pv(kt, bkt, qt, bqt, q0, vt, bvt, u, accs):
            nkb = (q0 + 512) // 128
            for kb in range(nkb):
                j = max(0, kb - q0 // 128)
                c0 = j * 128
                N = 512 - c0
                diag = kb * 128 >= q0
                pst, bps = pS.next()
                P.mm(pst[:, 0:N], kt[:, kb * 128:(kb + 1) * 128], qt[:, c0:512], start=True, stop=not diag,
                     reads=[bkt, bqt], writes=[bps])
                if diag:
                    P.mm(pst[:, 0:128], C.identb[:], C.cmaskb[:], start=False, stop=True, reads=[C.b_const], writes=[bps])
                pt, bpt = pts.next()
                P.op("act", lambda e, pt=pt, pst=pst, N=N, kb=kb: e.activation(pt[:, 0:N], pst[:, 0:N], AF.Exp, bias=biasv[:, kb, u:u + 1], scale=1.0),
                     reads=[bps, b_bias], writes=[bpt])
                for (acc, bacc, lhs) in accs:
                    l = lhs(kb)
                    P.mm(acc[0:l.shape[1], c0:512], l, pt[:, 0:N], start=(kb == 0), stop=(kb == nkb - 1),
                         reads=[bpt, bvt, C.b_const], writes=[bacc])

        for u in range(nu):
            vt, bvt = vts.next()
            if fox:
                P.dma("sp", vt[:], C.fv[:, u, :].rearrange("(b p) d -> p b d", p=128), writes=[bvt])
            else:
                P.dma("sp", vt[:], C.dv[:, u, :].rearrange("(b p) d -> p b d", p=128), writes=[bvt])
            maps = [u] if fox else [2 * u, 2 * u + 1]
            ktl = []
            for m in maps:
                kt, bkt = kts.next()
                P.dma("sp", kt[:], kd[m], reads=[b_kd], writes=[bkt])
                ktl.append((kt, bkt))
            for qi in range(NQ):
                q0 = qi * 512
                zt, bzt = zts.next()
                res = []
                for mi, m in enumerate(maps):
                    qt, bqt = qts.next()
                    P.dma("sp", qt[:], qd[m, :, q0:q0 + 512], reads=[b_qd], writes=[bqt])
                    kt, bkt = ktl[mi]
                    if fox:
                        po, bpo = pO.next()
                        accs = [(po, bpo, lambda kb, vt=vt: vt[:, kb, :])]
                    else:
                        po, bpo = pO.next()
                        pz, bpz = pO.next()
                        accs = [(po, bpo, lambda kb, vt=vt: vt[:, kb, :]), (pz, bpz, lambda kb: C.onesb[:])]
                    scores_and_pv(kt, bkt, qt, bqt, q0, vt, bvt, u, accs)
                    res.append(accs)
                if fox:
                    po, bpo, _ = res[0][0]
                    P.dma("sp", zt[0:64, :], C.zT[ch0 + u * 64: ch0 + (u + 1) * 64, q0:q0 + 512], writes=[bzt])
                    o, bo = osb.next()
                    P.op("act", lambda e, o=o, po=po: e.copy(o[0:65, :], po[0:65, :]), reads=[bpo], writes=[bo])
                    r, br = rsb.next()
                    P.op("dve", lambda e, r=r, o=o: e.reciprocal(r[64:65, :], o[64:65, :]), reads=[bo], writes=[br])
                    P.mm(pmisc[0:64, :], prm_ap(C, "ones", rows=(64, 65), c1=64), r[64:65, :], reads=[br, C.b_prm], writes=[b_pm])
                    o2, bo2 = osb.next()
                    P.op("dve", lambda e, o2=o2, o=o: e.tensor_tensor(o2[0:64, :], o[0:64, :], pmisc[0:64, :], ALU.mult), reads=[bo, b_pm], writes=[bo2])
                    y, by = ybf.next()
                    P.op("pool", lambda e, y=y, o2=o2, zt=zt: e.tensor_tensor(y[0:64, :], o2[0:64, :], zt[0:64, :], ALU.mult), reads=[bo2, bzt], writes=[by])
                    C.yz_w.append(P.dma("sp", C.yz[ch0 + u * 64: ch0 + (u + 1) * 64, q0:q0 + 512], y[0:64, :], reads=[by]))
                else:
                    P.dma("sp", zt[:], C.zT[ch0 + u * 128: ch0 + (u + 1) * 128, q0:q0 + 512], writes=[bzt])
                    ts = []
                    for accs in res:
                        (po, bpo, _), (pz, bpz, _) = accs
                        r, br = rsb.next()
                        P.op("dve", lambda e, r=r, pz=pz: e.reciprocal(r[:], pz[:]), reads=[bpz], writes=[br])
                        o, bo = osb.next()
                        P.op("dve", lambda e, o=o, po=po, r=r: e.tensor_tensor(o[:], po[:], r[:], ALU.mult), reads=[bpo, br], writes=[bo])
                        ts.append((o, bo))
                    (o1, bo1), (o2, bo2) = ts
                    od, bod = osb.next()
                    P.op("dve", lambda e, od=od, o1=o1, o2=o2: e.scalar_tensor_tensor(od[:], o2[:], lam[:, 3:4], o1[:], ALU.mult, ALU.add),
                         reads=[bo1, bo2, b_lam], writes=[bod])
                    s3, bs3 = sq3.next()
                    P.op("act", lambda e, s3=s3, od=od: e.activation(s3[:], od[:], AF.Square), reads=[bod], writes=[bs3])
                    P.mm(pmisc[:], C.onesb[:], s3[:], reads=[bs3, C.b_const], writes=[b_pm])
                    r, br = rsb.next()
                    rsqrt_from(C, r[:], pmisc[:], 1.0 / 128, 1e-5, b_pm, br)
                    o3, bo3 = osb.next()
                    P.op("dve", lambda e, o3=o3, od=od, r=r: e.scalar_tensor_tensor(o3[:], od[:], gsub[:], r[:], ALU.mult, ALU.mult),
                         reads=[bod, br, b_lam], writes=[bo3])
                    y, by = ybf.next()
                    P.op("pool", lambda e, y=y, o3=o3, zt=zt: e.tensor_tensor(y[:], o3[:], zt[:], ALU.mult), reads=[bo3, bzt], writes=[by])
                    C.yz_w.append(P.dma("sp", C.yz[ch0 + u * 128: ch0 + (u + 1) * 128, q0:q0 + 512], y[:], reads=[by]))


def phase_outproj(C):
    nc, P, S = C.nc, C.P, C.S
    TW = 1024
    outs = []
    with contextlib.ExitStack() as st:
        sb = lambda name, shape, dt: st.enter_context(nc.sbuf_tensor(un(name), list(shape), dt))
        ps = lambda name: st.enter_context(nc.psum_tensor(un(name), [128, 512], F32))
        yzt = sb("yzt", [128, 32, TW], BF16)
        b_yz = Buf()
        wst = Rot([sb(f"wo_st{i}", [128, 32, 128], F32) for i in range(2)])
        wbf = Rot([sb(f"wo_bf{i}", [128, 32, 128], BF16) for i in range(2)])
        xin = Rot([sb(f"xin{i}", [128, 512], F32) for i in range(3)])
        xo = Rot([sb(f"xo{i}", [128, 512], F32) for i in range(3)])
        pacc = Rot([ps(f"po{i}") for i in range(4)])
        for tt in range(S // TW):
            t0 = tt * TW
            for q in range(4):
                P.dma("sp", yzt[:, q * 8:(q + 1) * 8, :], C.yzp[q * 1024:(q + 1) * 1024, t0:t0 + TW].rearrange("(c p) t -> p c t", p=128), writes=[b_yz])
            nxt = None
            for dc in range(16):
                if nxt is None:
                    w, bw = wst.next()
                    P.dma("sp", w[:], C.wout[dc], writes=[bw])
                else:
                    w, bw = nxt
                wb, bwb = wbf.next()
                P.op("pool", lambda e, wb=wb, w=w: e.tensor_copy(wb[:], w[:]), reads=[bw], writes=[bwb])
                if dc + 1 < 16:
                    w2, bw2 = wst.next()
                    P.dma("sp", w2[:], C.wout[dc + 1], writes=[bw2])
                    nxt = (w2, bw2)
                else:
                    nxt = None
                for hf in range(TW // 512):
                    tl = hf * 512
                    xi, bxi = xin.next()
                    P.dma("act", xi[:], C.xT_in[dc * 128:(dc + 1) * 128, t0 + tl:t0 + tl + 512], writes=[bxi])
                    pa, bpa = pacc.next()
                    for mc in range(32):
                        P.mm(pa[:], wb[:, mc, :], yzt[:, mc, tl:tl + 512], start=(mc == 0), stop=(mc == 31), reads=[bwb, b_yz], writes=[bpa])
                    o, bo = xo.next()
                    P.op("dve", lambda e, o=o, pa=pa, xi=xi: e.tensor_tensor(o[:], pa[:], xi[:], ALU.add), reads=[bpa, bxi], writes=[bo])
                    outs.append(P.dma("act", C.xT_out[dc * 128:(dc + 1) * 128, t0 + tl:t0 + tl + 512], o[:], reads=[bo]))
    return outs


def core_inputs_front(inp, l, b, hh, S, xT):
    cols = core_columns(hh)
    W = np.asarray(inp["w_in"][l], np.float32)
    NG = len(cols)
    win = np.zeros((NG, 128, 16, 128), np.float32)
    for gi, c in enumerate(cols):
        wg = W[:, c]
        win[gi, :, :, :len(c)] = wg.reshape(16, 128, len(c)).transpose(1, 0, 2)
    pk = pack_params(inp, l, hh, S)
    slopes = 2.0 ** (-8.0 * np.arange(1, 9) / 8)
    alibi = (slopes[hh * 4: hh * 4 + 4, None] * np.arange(S, dtype=np.float64)[None, :]).astype(np.float32)
    return dict(xT=np.ascontiguousarray(xT), win=win, prm=pk.build(), alibi=alibi), pk.layout()


def wout_layout(inp, l):
    W = np.asarray(inp["w_out"][l], np.float32)
    perm = np.concatenate([my_channels(0), my_channels(1)])
    Wp = W[perm]
    return np.ascontiguousarray(Wp.reshape(32, 128, 16, 128).transpose(2, 1, 0, 3))


_PROGS = {}


def get_prog(S, has_prev, front, layout):
    key = (S, has_prev, front)
    if key not in _PROGS:
        _PROGS[key] = build_program(S, 1, has_prev, front, layout)[0]
    return _PROGS[key]


def kernel(**inp):
    inp = {k: np.asarray(v) for k, v in inp.items()}
    x = inp["x"]
    B, S, D = x.shape
    xT = [np.ascontiguousarray(x[b].T) for b in range(B)]
    yz_prev = None
    layout = None
    for l in range(DEPTH + 1):
        has_prev = l > 0
        front = l < DEPTH
        in_maps = []
        wout = wout_layout(inp, l - 1) if has_prev else None
        for core in range(8):
            b, hh = core // 2, core % 2
            if front:
                m, layout = core_inputs_front(inp, l, b, hh, S, xT[b])
            else:
                m = dict(xT=xT[b])
            if has_prev:
                m["yzp"] = yz_prev[b]
                m["wout"] = wout
            in_maps.append(m)
        nc = get_prog(S, has_prev, front, layout)
        res = run_bass_kernel_spmd(nc, in_maps, core_ids=list(range(8))).results
        if has_prev:
            xT = [np.asarray(res[2 * b]["xTo"]) for b in range(B)]
        if front:
            yz_prev = [np.concatenate([np.asarray(res[2 * b]["yz"]), np.asarray(res[2 * b + 1]["yz"])], 0) for b in range(B)]
    return np.stack([xT[b].T for b in range(B)], 0).astype(np.float32)


class TB:
    def __init__(self, t):
        self.t = t
        self.b = Buf()


class Pool2:
    def __init__(self, C, st):
        self.C, self.st, self.r = C, st, {}

    def get(self, name, shape, dt, n=2, psum=False):
        key = name
        if key not in self.r:
            nc = self.C.nc
            mk = (lambda: self.st.enter_context(nc.psum_tensor(un(name), list(shape), dt))) if psum else \
                 (lambda: self.st.enter_context(nc.sbuf_tensor(un(name), list(shape), dt)))
            self.r[key] = (0, [TB(mk()) for _ in range(n)])
        i, lst = self.r[key]
        self.r[key] = (i + 1, lst)
        return lst[i % len(lst)]


def v3(ap, c=8):
    return ap.rearrange("p (c t) -> p c t", c=c)


def doubling_inverse(C, pl, X, XT, m, pmr):
    P = C.P
    Pm32 = pl.get("dPm32", [64, 8, 64], F32)
    Pmb = pl.get("dPmb", [64, 8, 64], BF16, n=3)
    P.op("dve", lambda e: e.tensor_tensor(Pm32.t[:], X.t[:], C.ident8[0:64], ALU.add), reads=[X.b, C.b_const], writes=[Pm32.b])
    P.op("act", lambda e, Pmb=Pmb: e.copy(Pmb.t[:], Pm32.t[:]), reads=[Pm32.b], writes=[Pmb.b])
    A, AT = X, XT
    for k in range(5):
        last = k == 4
        pT = pmr()
        for c in range(8):
            P.mm(pT.t[0:64, c * 64:(c + 1) * 64], A.t[:, c, :], AT.t[:, c, :], reads=[A.b, AT.b], writes=[pT.b])
        A2T = pl.get("dA2T", [64, 8, 64], BF16, n=3)
        P.op("act", lambda e, A2T=A2T, pT=pT: e.copy(A2T.t[:], v3(pT.t[0:64, :])), reads=[pT.b], writes=[A2T.b])
        if not last:
            pA = pmr()
            for c in range(8):
                P.mm(pA.t[0:64, c * 64:(c + 1) * 64], AT.t[:, c, :], A.t[:, c, :], reads=[A.b, AT.b], writes=[pA.b])
            A2 = pl.get("dA2", [64, 8, 64], BF16, n=3)
            P.op("dve", lambda e, A2=A2, pA=pA: e.tensor_copy(A2.t[:], v3(pA.t[0:64, :])), reads=[pA.b], writes=[A2.b])
        pP = pmr()
        for c in range(8):
            P.mm(pP.t[0:64, c * 64:(c + 1) * 64], A2T.t[:, c, :], Pmb.t[:, c, :], reads=[A2T.b, Pmb.b], writes=[pP.b])
        P.op("dve", lambda e, pP=pP: e.tensor_tensor(Pm32.t[:], v3(pP.t[0:64, :]), Pm32.t[:], ALU.add), reads=[pP.b, Pm32.b], writes=[Pm32.b])
        Pmb = pl.get("dPmb", [64, 8, 64], BF16, n=3)
        P.op("act", lambda e, Pmb=Pmb: e.copy(Pmb.t[:], Pm32.t[:]), reads=[Pm32.b], writes=[Pmb.b])
        if not last:
            A, AT = A2, A2T
    return Pmb


def setup_chunk_consts(C):
    P = C.P
    sb = C.sb
    C.ident8 = sb("ident8", [128, 8, 64], F32)
    C.m01 = {}
    for nm in ("SU", "SL", "IU"):
        C.m01[nm] = sb("m01" + nm, [64, 8, 64], F32)
    C.mneg = {}
    for nm in ("MUI", "MUS", "MLS"):
        C.mneg[nm] = sb("mneg" + nm, [64, 512], BF16)
    C.rmask = sb("rmask", [128, 512], F32)
    for c in range(8):
        P.op("pool", lambda e, c=c: e.tensor_copy(C.ident8[0:64, c, :], prm_ap(C, "ident", rows=(0, 64), c1=64)), reads=[C.b_prm], writes=[C.b_const])
        for nm in ("SU", "SL", "IU"):
            P.op("pool", lambda e, c=c, nm=nm: e.tensor_copy(C.m01[nm][:, c, :], prm_ap(C, "m" + nm)), reads=[C.b_prm], writes=[C.b_const])
        for nm in ("MUI", "MUS", "MLS"):
            P.op("pool", lambda e, c=c, nm=nm: e.tensor_copy(C.mneg[nm][:, c * 64:(c + 1) * 64], prm_ap(C, "n" + nm)), reads=[C.b_prm], writes=[C.b_const])
        P.op("pool", lambda e, c=c: e.tensor_copy(C.rmask[:, c * 64:(c + 1) * 64], prm_ap(C, "rmask")), reads=[C.b_prm], writes=[C.b_const])


def phase_rwkv(C):
    nc, P, S = C.nc, C.P, C.S
    NS = S // 512
    ones64 = C.onesb[0:64, 0:64]
    ident64 = C.identb[0:64, 0:64]
    with contextlib.ExitStack() as st:
        pl = Pool2(C, st)
        sbp = lambda name, shape, dt: st.enter_context(nc.sbuf_tensor(un(name), list(shape), dt))
        H32 = TB(sbp("H32", [64, 8, 64], F32))
        Hb = [TB(sbp(f"Hb{h}", [64, 64], BF16)) for h in range(8)]
        Hbufs = [Buf() for _ in range(8)]
        P.op("dve", lambda e: e.memset(H32.t[:], 0.0), writes=Hbufs)
        for h in range(8):
            P.op("pool", lambda e, h=h: e.memset(Hb[h].t[:], 0.0), writes=[Hb[h].b])
        pw = lambda: pl.get("pw", [128, 512], F32, n=2, psum=True)
        pm = lambda: pl.get("pm", [128, 512], F32, n=2, psum=True)
        ptr = lambda: pl.get("ptr", [128, 1024], BF16, n=1, psum=True)
        pseq = pl.get("pseq", [128, 512], F32, n=1, psum=True)
        pY = lambda: pl.get("pY", [128, 512], F32, n=1, psum=True)
        T = lambda name, dt=F32, n=2: pl.get(name, [64, 512], dt, n=n)
        wupb, aupb = T("wupb", BF16, 1), T("aupb", BF16, 1)
        P.op("dve", lambda e: e.tensor_copy(wupb.t[:], prm_ap(C, "w_up")), reads=[C.b_prm], writes=[wupb.b])
        P.op("dve", lambda e: e.tensor_copy(aupb.t[:], prm_ap(C, "a_up")), reads=[C.b_prm], writes=[aupb.b])
        for s_ in range(NS):
            t0 = s_ * 512
            wlo = T("wlo")
            alo = T("alo")
            P.dma("sp", wlo.t[:], C.rwkvT[1536:1600, t0:t0 + 512], writes=[wlo.b])
            P.dma("sp", alo.t[:], C.rwkvT[1600:1664, t0:t0 + 512], writes=[alo.b])
            wlob, alob = T("wlob", BF16), T("alob", BF16)
            P.op("act", lambda e, wlo=wlo, wlob=wlob: e.activation(wlob.t[:], wlo.t[:], AF.Tanh), reads=[wlo.b], writes=[wlob.b])
            P.op("dve", lambda e, alo=alo, alob=alob: e.tensor_copy(alob.t[:], alo.t[:]), reads=[alo.b], writes=[alob.b])
            import os as _os
            for h in range(int(_os.environ.get("RWKV_NH", "8"))):
                r, k, v, zt = T("r"), T("k"), T("v"), T("z")
                for tl, base in ((r, 0), (k, 512), (v, 1024)):
                    P.dma("sp", tl.t[:], C.rwkvT[base + h * 64: base + (h + 1) * 64, t0:t0 + 512], writes=[tl.b])
                P.dma("sp", zt.t[:], C.zT[h * 64:(h + 1) * 64, t0:t0 + 512], writes=[zt.b])
                col = lambda nm, h=h: prm_ap(C, nm, c0=h, c1=h + 1)
                p1 = pw()
                P.mm(p1.t[0:64, :], wupb.t[:, h * 64:(h + 1) * 64], wlob.t[:], reads=[wlob.b, wupb.b], writes=[p1.b])
                lw = T("lw")
                P.op("act", lambda e, lw=lw, p1=p1, col=col: e.activation(lw.t[:], p1.t[0:64, :], AF.Sigmoid, bias=col("w0")), reads=[p1.b, C.b_prm], writes=[lw.b])
                P.op("pool", lambda e, lw=lw: e.tensor_scalar_mul(lw.t[:], lw.t[:], -math.exp(-0.5)), reads=[lw.b], writes=[lw.b])
                p2 = pw()
                P.mm(p2.t[0:64, :], aupb.t[:, h * 64:(h + 1) * 64], alob.t[:], reads=[alob.b, aupb.b], writes=[p2.b])
                a = T("a")
                P.op("act", lambda e, a=a, p2=p2, col=col: e.activation(a.t[:], p2.t[0:64, :], AF.Sigmoid, bias=col("a0")), reads=[p2.b, C.b_prm], writes=[a.b])
                kkr = T("kkr")
                P.op("dve", lambda e, kkr=kkr, k=k, col=col: e.tensor_scalar_mul(kkr.t[:], k.t[:], col("k_k")), reads=[k.b, C.b_prm], writes=[kkr.b])
                sq = T("sq", BF16)
                P.op("act", lambda e, sq=sq, kkr=kkr: e.activation(sq.t[:], kkr.t[:], AF.Square), reads=[kkr.b], writes=[sq.b])
                p3 = pw()
                P.mm(p3.t[0:64, :], ones64, sq.t[:], reads=[sq.b, C.b_const], writes=[p3.b])
                rn = T("rn")
                rsqrt_from(C, rn.t[:], p3.t[0:64, :], 1.0, 1e-6, p3.b, rn.b)
                kk = T("kk")
                P.op("dve", lambda e, kk=kk, kkr=kkr, rn=rn: e.tensor_tensor(kk.t[:], kkr.t[:], rn.t[:], ALU.mult), reads=[kkr.b, rn.b], writes=[kk.b])
                t1 = T("t1")
                P.op("dve", lambda e, t1=t1, a=a, col=col: e.tensor_scalar(t1.t[:], a.t[:], -1.0, col("k_a"), ALU.add, ALU.mult), reads=[a.b, C.b_prm], writes=[t1.b])
                km = T("km")
                P.op("dve", lambda e, km=km, t1=t1, k=k: e.scalar_tensor_tensor(km.t[:], t1.t[:], 1.0, k.t[:], ALU.add, ALU.mult), reads=[t1.b, k.b], writes=[km.b])
                beta = T("beta")
                P.op("pool", lambda e, beta=beta, kk=kk, a=a: e.tensor_tensor(beta.t[:], kk.t[:], a.t[:], ALU.mult), reads=[kk.b, a.b], writes=[beta.b])
                L = T("L")
                P.op("dve", lambda e, L=L, lw=lw: e.tensor_tensor_scan(L.t[:], C.rmask[0:64, :], lw.t[:], 0.0, ALU.mult, ALU.add), reads=[lw.b, C.b_const], writes=[L.b])
                eL, enL, eLm = T("eL"), T("enL"), T("eLm")
                P.op("act", lambda e, eL=eL, L=L: e.activation(eL.t[:], L.t[:], AF.Exp), reads=[L.b], writes=[eL.b])
                P.op("act", lambda e, enL=enL, L=L: e.activation(enL.t[:], L.t[:], AF.Exp, scale=-1.0), reads=[L.b], writes=[enL.b])
                P.op("pool", lambda e, eLm=eLm, L=L, lw=lw: e.tensor_tensor(eLm.t[:], L.t[:], lw.t[:], ALU.subtract), reads=[L.b, lw.b], writes=[eLm.b])
                P.op("act", lambda e, eLm=eLm: e.activation(eLm.t[:], eLm.t[:], AF.Exp), reads=[eLm.b], writes=[eLm.b])
                At, Bt, Kt, Rt = T("At", BF16), T("Bt", BF16), T("Kt", BF16), T("Rt", BF16)
                Bf, Kf = T("Bf"), T("Kf")
                P.op("dve", lambda e, At=At, kk=kk, eLm=eLm: e.scalar_tensor_tensor(At.t[:], kk.t[:], -1.0, eLm.t[:], ALU.mult, ALU.mult), reads=[kk.b, eLm.b], writes=[At.b])
                P.op("dve", lambda e, Bf=Bf, beta=beta, enL=enL: e.tensor_tensor(Bf.t[:], beta.t[:], enL.t[:], ALU.mult), reads=[beta.b, enL.b], writes=[Bf.b])
                P.op("pool", lambda e, Kf=Kf, km=km, enL=enL: e.tensor_tensor(Kf.t[:], km.t[:], enL.t[:], ALU.mult), reads=[km.b, enL.b], writes=[Kf.b])
                P.op("act", lambda e, Bt=Bt, Bf=Bf: e.copy(Bt.t[:], Bf.t[:]), reads=[Bf.b], writes=[Bt.b])
                P.op("act", lambda e, Kt=Kt, Kf=Kf: e.copy(Kt.t[:], Kf.t[:]), reads=[Kf.b], writes=[Kt.b])
                P.op("dve", lambda e, Rt=Rt, r=r, eL=eL: e.tensor_tensor(Rt.t[:], r.t[:], eL.t[:], ALU.mult), reads=[r.b, eL.b], writes=[Rt.b])
                Bh, Kh, Vb = T("Bh", BF16), T("Kh", BF16), T("Vb", BF16)
                for c in range(8):
                    Dc = eL.t[:, c * 64 + 63: c * 64 + 64]
                    cs = slice(c * 64, (c + 1) * 64)
                    P.op("dve", lambda e, Bh=Bh, Bf=Bf, Dc=Dc, cs=cs: e.tensor_scalar_mul(Bh.t[:, cs], Bf.t[:, cs], Dc), reads=[Bf.b, eL.b], writes=[Bh.b])
                    P.op("pool", lambda e, Kh=Kh, Kf=Kf, Dc=Dc, cs=cs: e.tensor_scalar_mul(Kh.t[:, cs], Kf.t[:, cs], Dc), reads=[Kf.b, eL.b], writes=[Kh.b])
                P.op("act", lambda e, Vb=Vb, v=v: e.copy(Vb.t[:], v.t[:]), reads=[v.b], writes=[Vb.b])
                rkr = T("rkr", BF16)
                P.op("dve", lambda e, rkr=rkr, r=r, km=km, col=col: e.scalar_tensor_tensor(rkr.t[:], r.t[:], col("r_k"), km.t[:], ALU.mult, ALU.mult), reads=[r.b, km.b, C.b_prm], writes=[rkr.b])
                p4 = pw()
                P.mm(p4.t[0:64, :], ones64, rkr.t[:], reads=[rkr.b, C.b_const], writes=[p4.b])
                bonus = T("bonus")
                P.op("dve", lambda e, bonus=bonus, p4=p4, v=v: e.tensor_tensor(bonus.t[:], p4.t[0:64, :], v.t[:], ALU.mult), reads=[p4.b, v.b], writes=[bonus.b])
                import os as _os
                _stop = int(_os.environ.get("RWKV_STOP", "9"))
                if _stop <= 1:
                    C.yz_w.append(P.dma("sp", C.yz[h * 64:(h + 1) * 64, t0:t0 + 512], Rt.t[:], reads=[Rt.b]))
                    continue
                tms = {}
                for nm, src in (("At", At), ("Bh", Bh), ("Kh", Kh), ("Vb", Vb)):
                    pt_ = ptr()
                    for c in range(8):
                        P.op("pe", lambda e, pt_=pt_, src=src, c=c: e.transpose(pt_.t[0:64, c * 64:(c + 1) * 64], src.t[:, c * 64:(c + 1) * 64], ident64),
                             reads=[src.b, C.b_const], writes=[pt_.b])
                    tm = pl.get("tm" + nm, [64, 8, 64], BF16)
                    P.op("act" if nm in ("At", "Kh") else "dve", (lambda e, tm=tm, pt_=pt_: e.copy(tm.t[:], v3(pt_.t[0:64, 0:512]))) if nm in ("At", "Kh") else
                         (lambda e, tm=tm, pt_=pt_: e.tensor_copy(tm.t[:], v3(pt_.t[0:64, 0:512]))), reads=[pt_.b], writes=[tm.b])
                    tms[nm] = tm
                if _stop <= 2:
                    C.yz_w.append(P.dma("sp", C.yz[h * 64:(h + 1) * 64, t0:t0 + 512], Rt.t[:], reads=[Rt.b]))
                    continue
                def cmat(name, lh, rh, mask, eng):
                    pp = pm()
                    for c in range(8):
                        cs = slice(c * 64, (c + 1) * 64)
                        P.mm(pp.t[0:64, cs], lh.t[:, cs], rh.t[:, cs], reads=[lh.b, rh.b], writes=[pp.b])
                    o = pl.get("cm" + name, [64, 8, 64], BF16)
                    P.op(eng, lambda e, o=o, pp=pp, mask=mask: e.tensor_tensor(o.t[:], v3(pp.t[0:64, :]), C.m01[mask][:], ALU.mult), reads=[pp.b, C.b_const], writes=[o.b])
                    return o
                X = cmat("X", Bt, At, "SU", "dve")
                XT = cmat("XT", At, Bt, "SL", "dve")
                Aak = cmat("Aak", At, Kt, "SL", "dve")
                ArbT = cmat("ArbT", Bt, Rt, "IU", "dve")
                ArkT = cmat("ArkT", Kt, Rt, "IU", "dve")
                if _stop <= 3:
                    C.yz_w.append(P.dma("sp", C.yz[h * 64:(h + 1) * 64, t0:t0 + 512], Rt.t[:], reads=[Rt.b]))
                    continue
                TT = doubling_inverse(C, pl, X, XT, None, pm)
                if _stop <= 4:
                    C.yz_w.append(P.dma("sp", C.yz[h * 64:(h + 1) * 64, t0:t0 + 512], Rt.t[:], reads=[Rt.b]))
                    continue
                pp = pm()
                for c in range(8):
                    P.mm(pp.t[0:64, c * 64:(c + 1) * 64], tms["At"].t[:, c, :], TT.t[:, c, :], reads=[tms["At"].b, TT.b], writes=[pp.b])
                WmT = pl.get("WmT", [64, 8, 64], BF16)
                P.op("act", lambda e, WmT=WmT, pp=pp: e.copy(WmT.t[:], v3(pp.t[0:64, :])), reads=[pp.b], writes=[WmT.b])
                pp2 = pm()
                for c in range(8):
                    P.mm(pp2.t[0:64, c * 64:(c + 1) * 64], Aak.t[:, c, :], TT.t[:, c, :], reads=[Aak.b, TT.b], writes=[pp2.b])
                TAT = pl.get("TAT", [64, 8, 64], BF16)
                P.op("dve", lambda e, TAT=TAT, pp2=pp2: e.tensor_copy(TAT.t[:], v3(pp2.t[0:64, :])), reads=[pp2.b], writes=[TAT.b])
                if _stop <= 5:
                    C.yz_w.append(P.dma("sp", C.yz[h * 64:(h + 1) * 64, t0:t0 + 512], Rt.t[:], reads=[Rt.b]))
                    continue
                py = pY()
                Vtm, Bhtm, Khtm = tms["Vb"], tms["Bh"], tms["Kh"]
                for c in range(8):
                    cs = slice(c * 64, (c + 1) * 64)
                    P.mm(pseq.t[0:64, 0:64], WmT.t[:, c, :], Hb[h].t[:], start=True, stop=False, reads=[WmT.b, Hb[h].b], writes=[pseq.b])
                    P.mm(pseq.t[0:64, 0:64], TAT.t[:, c, :], Vtm.t[:, c, :], start=False, stop=True, reads=[TAT.b, Vtm.b], writes=[pseq.b])
                    Ub = pl.get("Ub", [64, 64], BF16, n=3)
                    P.op("act", lambda e, Ub=Ub: e.copy(Ub.t[:], pseq.t[0:64, 0:64]), reads=[pseq.b], writes=[Ub.b])
                    P.mm(py.t[0:64, cs], Hb[h].t[:], Rt.t[:, cs], start=True, stop=False, reads=[Hb[h].b, Rt.b], writes=[py.b])
                    P.mm(py.t[0:64, cs], Ub.t[:], ArbT.t[:, c, :], start=False, stop=False, reads=[Ub.b, ArbT.b], writes=[py.b])
                    P.mm(py.t[0:64, cs], Vtm.t[:, c, :], ArkT.t[:, c, :], start=False, stop=True, reads=[Vtm.b, ArkT.b], writes=[py.b])
                    P.mm(pseq.t[0:64, 64:128], Bhtm.t[:, c, :], Ub.t[:], start=True, stop=False, reads=[Bhtm.b, Ub.b], writes=[pseq.b])
                    P.mm(pseq.t[0:64, 64:128], Khtm.t[:, c, :], Vtm.t[:, c, :], start=False, stop=True, reads=[Khtm.b, Vtm.b], writes=[pseq.b])
                    Dc = eL.t[:, c * 64 + 63: c * 64 + 64]
                    P.op("dve", lambda e, h=h, Dc=Dc: e.scalar_tensor_tensor(H32.t[:, h, :], H32.t[:, h, :], Dc, pseq.t[0:64, 64:128], ALU.mult, ALU.add),
                         reads=[pseq.b, eL.b, Hbufs[h]], writes=[Hbufs[h]])
                    P.op("act", lambda e, h=h: e.copy(Hb[h].t[:], H32.t[:, h, :]), reads=[Hbufs[h]], writes=[Hb[h].b])
                if _stop <= 6:
                    C.yz_w.append(P.dma("sp", C.yz[h * 64:(h + 1) * 64, t0:t0 + 512], Rt.t[:], reads=[Rt.b]))
                    continue
                Yf = T("Yf")
                Yb = T("Yb", BF16)
                P.op("act", lambda e, Yf=Yf, py=py: e.copy(Yf.t[:], py.t[0:64, :]), reads=[py.b], writes=[Yf.b])
                P.op("dve", lambda e, Yb=Yb, Yf=Yf: e.tensor_copy(Yb.t[:], Yf.t[:]), reads=[Yf.b], writes=[Yb.b])
                p5 = pw()
                P.mm(p5.t[0:64, :], ones64, Yb.t[:], reads=[Yb.b, C.b_const], writes=[p5.b])
                yc = T("yc")
                P.op("dve", lambda e, yc=yc, p5=p5, Yf=Yf: e.scalar_tensor_tensor(yc.t[:], p5.t[0:64, :], -1.0 / 64, Yf.t[:], ALU.mult, ALU.add), reads=[p5.b, Yf.b], writes=[yc.b])
                if _stop <= 7:
                    C.yz_w.append(P.dma("sp", C.yz[h * 64:(h + 1) * 64, t0:t0 + 512], Rt.t[:], reads=[Rt.b]))
                    continue
                sq2 = T("sq", BF16)
                P.op("act", lambda e, sq2=sq2, yc=yc: e.activation(sq2.t[:], yc.t[:], AF.Square), reads=[yc.b], writes=[sq2.b])
                p6 = pw()
                P.mm(p6.t[0:64, :], ones64, sq2.t[:], reads=[sq2.b, C.b_const], writes=[p6.b])
                rs = T("rn")
                rsqrt_from(C, rs.t[:], p6.t[0:64, :], 1.0 / 64, 64e-5, p6.b, rs.b)
                if _stop <= 8:
                    C.yz_w.append(P.dma("sp", C.yz[h * 64:(h + 1) * 64, t0:t0 + 512], Rt.t[:], reads=[Rt.b]))
                    continue
                yn = T("yn")
                P.op("dve", lambda e, yn=yn, yc=yc, rs=rs: e.tensor_tensor(yn.t[:], yc.t[:], rs.t[:], ALU.mult), reads=[yc.b, rs.b], writes=[yn.b])
                P.op("dve", lambda e, yn=yn, col=col: e.tensor_scalar(yn.t[:], yn.t[:], col("ln_g"), col("ln_b"), ALU.mult, ALU.add), reads=[yn.b, C.b_prm], writes=[yn.b])
                P.op("pool", lambda e, yn=yn, bonus=bonus: e.tensor_tensor(yn.t[:], yn.t[:], bonus.t[:], ALU.add), reads=[yn.b, bonus.b], writes=[yn.b])
                yo = T("yo", BF16)
                P.op("dve", lambda e, yo=yo, yn=yn, zt=zt: e.tensor_tensor(yo.t[:], yn.t[:], zt.t[:], ALU.mult), reads=[yn.b, zt.b], writes=[yo.b])
                C.yz_w.append(P.dma("sp", C.yz[h * 64:(h + 1) * 64, t0:t0 + 512], yo.t[:], reads=[yo.b]))
        C.sb_left_rwkv = nc.sbuf_bytes_remaining


def phase_gdn(C):
    nc, P, S = C.nc, C.P, C.S
    NS = S // 512
    ident64 = C.identb[0:64, 0:64]
    with contextlib.ExitStack() as st:
        pl = Pool2(C, st)
        sbp = lambda name, shape, dt: st.enter_context(nc.sbuf_tensor(un(name), list(shape), dt))
        S32 = [TB(sbp(f"S32_{h}", [128, 128], F32)) for h in range(4)]
        Sb = [TB(sbp(f"Sb_{h}", [128, 128], BF16)) for h in range(4)]
        for h in range(4):
            P.op("dve", lambda e, h=h: e.memset(S32[h].t[:], 0.0), writes=[S32[h].b])
            P.op("pool", lambda e, h=h: e.memset(Sb[h].t[:], 0.0), writes=[Sb[h].b])
        pw = lambda: pl.get("pw", [128, 512], F32, n=2, psum=True)
        pm = lambda: pl.get("pm", [128, 512], F32, n=2, psum=True)
        ptr = lambda: pl.get("ptr", [128, 1024], BF16, n=1, psum=True)
        pseq = pl.get("pseq", [128, 512], F32, n=1, psum=True)
        pO = lambda: pl.get("pO", [128, 512], F32, n=1, psum=True)
        T = lambda name, dt=F32, n=2: pl.get(name, [128, 512], dt, n=n)
        nea = TB(sbp("nea", [4, 1], F32))
        P.op("act", lambda e: e.activation(nea.t[:], prm_ap(C, "a_log"), AF.Exp), reads=[C.b_prm], writes=[nea.b])
        P.op("dve", lambda e: e.tensor_scalar_mul(nea.t[:], nea.t[:], -1.0), reads=[nea.b], writes=[nea.b])
        T4 = lambda name, n=2: pl.get(name, [8, 512], F32, n=n)
        for s_ in range(NS):
            t0 = s_ * 512
            cs5 = slice(t0, t0 + 512)
            g, beta, gc, G1, G2 = T4("g4"), T4("beta4"), T4("gc4"), T4("G1"), T4("G2")
            G2h = [T4(f"G2h{h}") for h in range(4)]
            P.dma("sp", g.t[0:4, :], C.miscT[1, 0:4, cs5], writes=[g.b])
            P.dma("sp", beta.t[0:4, :], C.miscT[2, 0:4, cs5], writes=[beta.b])
            P.op("act", lambda e, g=g: e.activation(g.t[0:4, :], g.t[0:4, :], AF.Exp, bias=prm_ap(C, "dt_b")), reads=[g.b, C.b_prm], writes=[g.b])
            P.op("act", lambda e, g=g: e.activation(g.t[0:4, :], g.t[0:4, :], AF.Ln, bias=C.epsc[0:4, 3:4]), reads=[g.b, C.b_const], writes=[g.b])
            P.op("dve", lambda e, g=g: e.tensor_scalar_mul(g.t[0:4, :], g.t[0:4, :], nea.t[:]), reads=[g.b, nea.b], writes=[g.b])
            P.op("act", lambda e, beta=beta: e.activation(beta.t[0:4, :], beta.t[0:4, :], AF.Sigmoid), reads=[beta.b], writes=[beta.b])
            P.op("dve", lambda e, g=g, gc=gc: e.tensor_tensor_scan(gc.t[0:4, :], C.rmask[0:4, :], g.t[0:4, :], 0.0, ALU.mult, ALU.add),
                 reads=[g.b, C.b_const], writes=[gc.b])
            p1 = pw()
            P.mm(p1.t[0:8, :], prm_ap(C, "E1"), gc.t[0:4, :], reads=[gc.b, C.b_prm], writes=[p1.b])
            P.op("act", lambda e, p1=p1, G1=G1: e.activation(G1.t[:], p1.t[0:8, :], AF.Identity, bias=prm_ap(C, "c1")), reads=[p1.b, C.b_prm], writes=[G1.b])
            p2 = pw()
            P.mm(p2.t[0:8, :], prm_ap(C, "E2"), gc.t[0:4, :], reads=[gc.b, C.b_prm], writes=[p2.b])
            P.op("act", lambda e, p2=p2, G2=G2: e.activation(G2.t[:], p2.t[0:8, :], AF.Identity, bias=prm_ap(C, "c2")), reads=[p2.b, C.b_prm], writes=[G2.b])
            for h in range(4):
                P.op("dve", lambda e, h=h, G2=G2, G2h=G2h: e.tensor_scalar_mul(G2h[h].t[:], G2.t[:], prm_ap(C, "hmask", c0=h, c1=h + 1)), reads=[G2.b, C.b_prm], writes=[G2h[h].b])
            for h in range(4):
                sel = prm_ap(C, "sel", c0=h * 128, c1=(h + 1) * 128)
                pgc = pw()
                P.mm(pgc.t[:], sel, gc.t[0:4, :], reads=[gc.b, C.b_prm], writes=[pgc.b])
                gcB = T("gcB")
                P.op("act", lambda e, gcB=gcB, pgc=pgc: e.copy(gcB.t[:], pgc.t[:]), reads=[pgc.b], writes=[gcB.b])
                eg = T("eg")
                P.op("act", lambda e, eg=eg, pgc=pgc: e.activation(eg.t[:], pgc.t[:], AF.Exp), reads=[pgc.b], writes=[eg.b])
                pbt = pw()
                P.mm(pbt.t[:], sel, beta.t[0:4, :], reads=[beta.b, C.b_prm], writes=[pbt.b])
                btB = T("btB")
                P.op("act", lambda e, btB=btB, pbt=pbt: e.copy(btB.t[:], pbt.t[:]), reads=[pbt.b], writes=[btB.b])
                eglc = T("eglc")
                P.op("pool", lambda e, eglc=eglc, gcB=gcB: e.tensor_tensor(v3(eglc.t[:]), v3(gcB.t[:])[:, :, 63:64].to_broadcast([128, 8, 64]), v3(gcB.t[:]), ALU.subtract),
                     reads=[gcB.b], writes=[eglc.b])
                P.op("act", lambda e, eglc=eglc: e.activation(eglc.t[:], eglc.t[:], AF.Exp), reads=[eglc.b], writes=[eglc.b])
                qkv = []
                for part in range(3):
                    raw = pl.get("raw", [128, 515], F32, n=3)
                    row0 = part * 512 + h * 128
                    if s_ == 0:
                        P.op("pool", lambda e, raw=raw: e.memset(raw.t[:, 0:3], 0.0), writes=[raw.b])
                        P.dma("sp", raw.t[:, 3:515], C.gdnT[row0:row0 + 128, 0:512], writes=[raw.b])
                    else:
                        P.dma("sp", raw.t[:], C.gdnT[row0:row0 + 128, t0 - 3:t0 + 512], writes=[raw.b])
                    gi = part * 4 + h
                    acc = T("cacc", n=3)
                    eng = "dve" if part != 1 else "pool"
                    P.op(eng, lambda e, acc=acc, raw=raw, gi=gi: e.tensor_scalar_mul(acc.t[:], raw.t[:, 0:512], prm_ap(C, "conv", c0=gi * 4, c1=gi * 4 + 1)),
                         reads=[raw.b, C.b_prm], writes=[acc.b])
                    for j in range(1, 4):
                        P.op("dve", lambda e, acc=acc, raw=raw, gi=gi, j=j: e.scalar_tensor_tensor(acc.t[:], raw.t[:, j:j + 512], prm_ap(C, "conv", c0=gi * 4 + j, c1=gi * 4 + j + 1), acc.t[:], ALU.mult, ALU.add),
                             reads=[raw.b, acc.b, C.b_prm], writes=[acc.b])
                    P.op("act", lambda e, acc=acc: e.activation(acc.t[:], acc.t[:], AF.Silu), reads=[acc.b], writes=[acc.b])
                    qkv.append(acc)
                q, k, v = qkv
                zt = T("z")
                P.dma("sp", zt.t[:], C.zT[1024 + h * 128: 1024 + (h + 1) * 128, cs5], writes=[zt.b])
                nrm = []
                for src, scl in ((q, 128 ** -0.5), (k, 1.0)):
                    sq = T("sq", BF16)
                    P.op("act", lambda e, sq=sq, src=src: e.activation(sq.t[:], src.t[:], AF.Square), reads=[src.b], writes=[sq.b])
                    pn = pw()
                    P.mm(pn.t[:], C.onesb[:], sq.t[:], reads=[sq.b, C.b_const], writes=[pn.b])
                    rn = T("rn")
                    rsqrt_from(C, rn.t[:], pn.t[:], 1.0, 1e-6, pn.b, rn.b)
                    o = T("nrm", n=3)
                    P.op("dve", lambda e, o=o, src=src, rn=rn, scl=scl: e.scalar_tensor_tensor(o.t[:], src.t[:], scl, rn.t[:], ALU.mult, ALU.mult), reads=[src.b, rn.b], writes=[o.b])
                    nrm.append(o)
                qh, kh = nrm
                Kbf = T("Kbf")
                P.op("dve", lambda e, Kbf=Kbf, kh=kh, btB=btB: e.tensor_tensor(Kbf.t[:], kh.t[:], btB.t[:], ALU.mult), reads=[kh.b, btB.b], writes=[Kbf.b])
                Kb, Khb, Qg, Qb = T("Kb", BF16), T("Khb", BF16), T("Qg", BF16), T("Qb", BF16)
                Kbg, Kd, Vbt = T("Kbg", BF16), T("Kd", BF16), T("Vbt", BF16)
                P.op("act", lambda e, Kb=Kb, Kbf=Kbf: e.copy(Kb.t[:], Kbf.t[:]), reads=[Kbf.b], writes=[Kb.b])
                P.op("act", lambda e, Khb=Khb, kh=kh: e.copy(Khb.t[:], kh.t[:]), reads=[kh.b], writes=[Khb.b])
                P.op("act", lambda e, Qb=Qb, qh=qh: e.copy(Qb.t[:], qh.t[:]), reads=[qh.b], writes=[Qb.b])
                P.op("pool", lambda e, Qg=Qg, qh=qh, eg=eg: e.tensor_tensor(Qg.t[:], qh.t[:], eg.t[:], ALU.mult), reads=[qh.b, eg.b], writes=[Qg.b])
                P.op("dve", lambda e, Kbg=Kbg, Kbf=Kbf, eg=eg: e.tensor_tensor(Kbg.t[:], Kbf.t[:], eg.t[:], ALU.mult), reads=[Kbf.b, eg.b], writes=[Kbg.b])
                P.op("pool", lambda e, Kd=Kd, kh=kh, eglc=eglc: e.tensor_tensor(Kd.t[:], kh.t[:], eglc.t[:], ALU.mult), reads=[kh.b, eglc.b], writes=[Kd.b])
                P.op("dve", lambda e, Vbt=Vbt, v=v, btB=btB: e.tensor_tensor(Vbt.t[:], v.t[:], btB.t[:], ALU.mult), reads=[v.b, btB.b], writes=[Vbt.b])
                tms = {}
                for nm, src in (("Kbg", Kbg), ("Kd", Kd), ("Vbt", Vbt)):
                    pt_ = ptr()
                    for c in range(8):
                        P.op("pe", lambda e, pt_=pt_, src=src, c=c: e.transpose(pt_.t[0:64, c * 128:(c + 1) * 128], src.t[:, c * 64:(c + 1) * 64], C.identb[:]),
                             reads=[src.b, C.b_const], writes=[pt_.b])
                    tm = pl.get("tm" + nm, [64, 8, 128], BF16)
                    if nm == "Kd":
                        P.op("dve", lambda e, tm=tm, pt_=pt_: e.tensor_copy(tm.t[:], pt_.t[0:64, :].rearrange("p (c t) -> p c t", c=8)), reads=[pt_.b], writes=[tm.b])
                    else:
                        P.op("act", lambda e, tm=tm, pt_=pt_: e.copy(tm.t[:], pt_.t[0:64, :].rearrange("p (c t) -> p c t", c=8)), reads=[pt_.b], writes=[tm.b])
                    tms[nm] = tm
                def decay(name, lh, rh, maskname, dt):
                    pp = pm()
                    P.mm(pp.t[0:64, :], ident64, C.mneg[maskname][:], start=True, stop=False, reads=[C.b_const], writes=[pp.b])
                    for c in range(8):
                        cs = slice(c * 64, (c + 1) * 64)
                        P.mm(pp.t[0:64, c * 64:(c + 1) * 64], lh.t[:, cs], rh.t[:, cs], start=False, stop=(c == 7), reads=[lh.b, rh.b], writes=[pp.b])
                    o = pl.get("dec" + name, [64, 512], dt)
                    P.op("act", lambda e, o=o, pp=pp: e.activation(o.t[:], pp.t[0:64, :], AF.Exp), reads=[pp.b], writes=[o.b])
                    return o
                DTi = decay("DTi", G2h[h], G1, "MUI", F32)
                DTs = decay("DTs", G2h[h], G1, "MUS", F32)
                Ds = decay("Ds", G1, G2h[h], "MLS", F32)

                def prod(name, lh, rh, dec, scl):
                    pp = pm()
                    for c in range(8):
                        cs = slice(c * 64, (c + 1) * 64)
                        P.mm(pp.t[0:64, cs], lh.t[:, cs], rh.t[:, cs], reads=[lh.b, rh.b], writes=[pp.b])
                    o = pl.get("pr" + name, [64, 8, 64], BF16)
                    P.op("dve", lambda e, o=o, pp=pp, dec=dec, scl=scl: e.scalar_tensor_tensor(o.t[:], v3(pp.t[0:64, :]), scl, v3(dec.t[:]), ALU.mult, ALU.mult),
                         reads=[pp.b, dec.b], writes=[o.b])
                    return o
                X = prod("X", Khb, Kb, DTs, -1.0)
                XT = prod("XT", Kb, Khb, Ds, -1.0)
                AiT = prod("AiT", Khb, Qb, DTi, 1.0)
                TT = doubling_inverse(C, pl, X, XT, None, pm)
                nWT = pl.get("nWT", [128, 8, 64], BF16)
                pp = pm()
                for c in range(8):
                    P.mm(pp.t[:, c * 64:(c + 1) * 64], tms["Kbg"].t[:, c, :], TT.t[:, c, :], reads=[tms["Kbg"].b, TT.b], writes=[pp.b])
                P.op("act", lambda e, nWT=nWT, pp=pp: e.activation(nWT.t[:], v3(pp.t[:]), AF.Copy, scale=-1.0), reads=[pp.b], writes=[nWT.b])
                po = pO()
                Vtm, Kdtm = tms["Vbt"], tms["Kd"]
                for c in range(8):
                    cs = slice(c * 64, (c + 1) * 64)
                    P.mm(pseq.t[0:64, 0:128], TT.t[:, c, :], Vtm.t[:, c, :], start=True, stop=False, reads=[TT.b, Vtm.b], writes=[pseq.b])
                    P.mm(pseq.t[0:64, 0:128], nWT.t[:, c, :], Sb[h].t[:], start=False, stop=True, reads=[nWT.b, Sb[h].b], writes=[pseq.b])
                    vnb = pl.get("vnb", [64, 128], BF16, n=3)
                    P.op("act", lambda e, vnb=vnb: e.copy(vnb.t[:], pseq.t[0:64, 0:128]), reads=[pseq.b], writes=[vnb.b])
                    P.mm(po.t[:, cs], Sb[h].t[:], Qg.t[:, cs], start=True, stop=False, reads=[Sb[h].b, Qg.b], writes=[po.b])
                    P.mm(po.t[:, cs], vnb.t[:], AiT.t[:, c, :], start=False, stop=True, reads=[vnb.b, AiT.b], writes=[po.b])
                    P.mm(pseq.t[:, 128:256], Kdtm.t[:, c, :], vnb.t[:], start=True, stop=True, reads=[Kdtm.b, vnb.b], writes=[pseq.b])
                    egl = eg.t[:, c * 64 + 63: c * 64 + 64]
                    P.op("dve", lambda e, h=h, egl=egl: e.scalar_tensor_tensor(S32[h].t[:], S32[h].t[:], egl, pseq.t[:, 128:256], ALU.mult, ALU.add),
                         reads=[pseq.b, eg.b, S32[h].b], writes=[S32[h].b])
                    P.op("act", lambda e, h=h: e.copy(Sb[h].t[:], S32[h].t[:]), reads=[S32[h].b], writes=[Sb[h].b])
                Of = T("Of")
                P.op("act", lambda e, Of=Of, po=po: e.copy(Of.t[:], po.t[:]), reads=[po.b], writes=[Of.b])
                sq = T("sq", BF16)
                P.op("act", lambda e, sq=sq, po=po: e.activation(sq.t[:], po.t[:], AF.Square), reads=[po.b], writes=[sq.b])
                pv_ = pw()
                P.mm(pv_.t[:], C.onesb[:], sq.t[:], reads=[sq.b, C.b_const], writes=[pv_.b])
                rs = T("rn")
                rsqrt_from(C, rs.t[:], pv_.t[:], 1.0 / 128, 1e-6, pv_.b, rs.b)
                yn = T("yn")
                P.op("dve", lambda e, yn=yn, Of=Of, rs=rs: e.scalar_tensor_tensor(yn.t[:], Of.t[:], prm_ap(C, "gnorm"), rs.t[:], ALU.mult, ALU.mult), reads=[Of.b, rs.b, C.b_prm], writes=[yn.b])
                yo = T("yo", BF16)
                P.op("pool", lambda e, yo=yo, yn=yn, zt=zt: e.tensor_tensor(yo.t[:], yn.t[:], zt.t[:], ALU.mult), reads=[yn.b, zt.b], writes=[yo.b])
                C.yz_w.append(P.dma("sp", C.yz[1024 + h * 128: 1024 + (h + 1) * 128, cs5], yo.t[:], reads=[yo.b]))
        C.sb_left_gdn = nc.sbuf_bytes_remaining
```
